# Optimizing a Trainium2 kernel written in Bass

```python
import math
import jax, jax.numpy as jnp
from jax import lax
import numpy as np

D_MODEL = 1024
BATCH = 8
SEQ = 8192
DEPTH = 1

GRID_W = 64
CTX_LEN = 256
HEAD_DIM = 64
ATTN_SCALE = HEAD_DIM ** -0.5
ROPE_FREQS = HEAD_DIM // 4
ROPE_BASE = 10000.0
EPS = 1e-6
BLOCK = 128
A_Q_HEADS = 16
A_KV_HEADS = 2
A_GROUP = A_Q_HEADS // A_KV_HEADS
WINDOW = 128
A_WIDTH = A_Q_HEADS * HEAD_DIM
B_HEADS = 8
B_V_DIM = 2 * HEAD_DIM
B_WIDTH = B_HEADS * B_V_DIM
KA_W = A_KV_HEADS * HEAD_DIM
KB_W = B_HEADS * 2 * HEAD_DIM
KV_W = 2 * KA_W + KB_W + B_WIDTH
IN_W = KV_W + A_WIDTH + KB_W + 2 * D_MODEL
N_EXPERTS = 256
TOP_K = 8
N_GROUPS = 8
TOPK_GROUPS = 4
D_EXPERT = D_MODEL // 4
ROUTED_SCALE = 2.5
MOE_BLOCK = 128

kernel_name = "hybrid_dit_gqa_sink_diffattn_moe"


def rms_norm(x, g):
    xf = x.astype(jnp.float32)
    y = xf * lax.rsqrt(jnp.mean(xf * xf, axis=-1, keepdims=True) + EPS)
    return (y * g.astype(jnp.float32)).astype(x.dtype)


def rope_tables(n):
    rows = n // GRID_W
    row = jnp.broadcast_to(jnp.arange(rows)[:, None], (rows, GRID_W)).reshape(-1)
    col = jnp.broadcast_to(jnp.arange(GRID_W)[None, :], (rows, GRID_W)).reshape(-1)
    freqs = ROPE_BASE ** (-jnp.arange(ROPE_FREQS, dtype=jnp.float32) / ROPE_FREQS)
    pos = jnp.stack([row, col], axis=-1).astype(jnp.float32)
    ang = pos[:, :, None] * freqs
    return jnp.cos(ang), jnp.sin(ang)


def apply_rope(x, cos, sin):
    bshape = (x.shape[1],) + (1,) * (x.ndim - 3) + (2, ROPE_FREQS)
    c = cos.reshape(bshape)
    s = sin.reshape(bshape)
    xr = x.astype(jnp.float32).reshape(x.shape[:-1] + (2, 2, ROPE_FREQS))
    x1, x2 = xr[..., 0, :], xr[..., 1, :]
    out = jnp.stack([x1 * c - x2 * s, x2 * c + x1 * s], axis=-2)
    return out.reshape(x.shape).astype(x.dtype)


def sink_attend(q, k, v, sink, mask=None):
    s = jnp.einsum('bqkgd,bskd->bkgqs', q, k).astype(jnp.float32) * ATTN_SCALE
    if mask is not None:
        s = jnp.where(mask, s, -jnp.inf)
    sink_col = jnp.broadcast_to(sink.astype(jnp.float32)[None, :, :, None, None], s.shape[:-1] + (1,))
    p = jax.nn.softmax(jnp.concatenate([s, sink_col], axis=-1), axis=-1)[..., :-1]
    return jnp.einsum('bkgqs,bskd->bqkgd', p.astype(v.dtype), v)


def window_attention(q, k, v, kc, vc, sink):
    B, n = q.shape[:2]
    nb = n // BLOCK
    span = BLOCK + 2 * WINDOW
    pad = ((0, 0), (WINDOW, WINDOW), (0, 0), (0, 0))
    k_pad = jnp.pad(k, pad)
    v_pad = jnp.pad(v, pad)
    qb = jnp.moveaxis(q.reshape((B, nb, BLOCK) + q.shape[2:]), 1, 0)
    offs = jnp.arange(span) - WINDOW
    band = jnp.abs(jnp.arange(BLOCK)[:, None] - offs[None, :]) <= WINDOW
    ctx_ok = jnp.ones((BLOCK, kc.shape[1]), dtype=bool)

    def one_block(args):
        i, q_i = args
        start = i * BLOCK
        k_i = jnp.concatenate([lax.dynamic_slice_in_dim(k_pad, start, span, axis=1), kc], axis=1)
        v_i = jnp.concatenate([lax.dynamic_slice_in_dim(v_pad, start, span, axis=1), vc], axis=1)
        kpos = start + offs
        valid = band & ((kpos >= 0) & (kpos < n))[None, :]
        mask = jnp.concatenate([valid, ctx_ok], axis=1)
        return sink_attend(q_i, k_i, v_i, sink, mask)

    out = lax.map(one_block, (jnp.arange(nb), qb))
    return jnp.moveaxis(out, 0, 1).reshape(B, n, A_WIDTH)


def diff_attend(q, k, v, lam):
    s = jnp.einsum('bqhjd,bshjd->bhjqs', q, k).astype(jnp.float32) * ATTN_SCALE
    p = jax.nn.softmax(s, axis=-1)
    a = p[:, :, 0] - lam * p[:, :, 1]
    return jnp.einsum('bhqs,bshe->bqhe', a.astype(v.dtype), v)


def blocked_diff_attention(q, k, v, lam):
    B, n = q.shape[:2]
    nb = n // BLOCK
    qb = jnp.moveaxis(q.reshape((B, nb, BLOCK) + q.shape[2:]), 1, 0)
    out = lax.map(lambda q_i: diff_attend(q_i, k, v, lam), qb)
    return jnp.moveaxis(out, 0, 1).reshape((B, n) + out.shape[3:])


def mixer_kv(kv, p):
    B, S = kv.shape[:2]
    ka, va, kb, vb = jnp.split(kv, [KA_W, 2 * KA_W, 2 * KA_W + KB_W], axis=-1)
    ka = rms_norm(ka.reshape(B, S, A_KV_HEADS, HEAD_DIM), p['knorm_a'])
    va = va.reshape(B, S, A_KV_HEADS, HEAD_DIM)
    kb = rms_norm(kb.reshape(B, S, B_HEADS, 2, HEAD_DIM), p['knorm_b'])
    vb = vb.reshape(B, S, B_HEADS, B_V_DIM)
    return ka, va, kb, vb


def mixer_q(qg, p):
    B, S = qg.shape[:2]
    qa, qb, ga, gb = jnp.split(qg, [A_WIDTH, A_WIDTH + KB_W, A_WIDTH + KB_W + D_MODEL], axis=-1)
    qa = rms_norm(qa.reshape(B, S, A_KV_HEADS, A_GROUP, HEAD_DIM), p['qnorm_a'])
    qb = rms_norm(qb.reshape(B, S, B_HEADS, 2, HEAD_DIM), p['qnorm_b'])
    return qa, qb, jax.nn.sigmoid(ga), jax.nn.sigmoid(gb)


def merge_branches(oa, ob, ga, gb, p, lam_init):
    B, S = oa.shape[:2]
    ob = rms_norm(ob, p['subln_g']) * (1.0 - lam_init)
    ya = oa.reshape(B, S, A_WIDTH) @ p['w_pa']
    yb = ob.reshape(B, S, B_WIDTH) @ p['w_pb']
    return (ga * ya + gb * yb) @ p['w_o']


def token_mixers(h, hc, cos, sin, p, lam_init, update_ctx):
    proj = h @ p['w_in']
    ka, va, kb, vb = mixer_kv(proj[..., :KV_W], p)
    qa, qb, ga, gb = mixer_q(proj[..., KV_W:], p)
    ka, kb, qa, qb = (apply_rope(t, cos, sin) for t in (ka, kb, qa, qb))
    proj_c = hc @ (p['w_in'] if update_ctx else p['w_in'][:, :KV_W])
    kac, vac, kbc, vbc = mixer_kv(proj_c[..., :KV_W], p)
    oa = window_attention(qa, ka, va, kac, vac, p['sink_a'])
    ob = blocked_diff_attention(qb, jnp.concatenate([kb, kbc], axis=1),
                                jnp.concatenate([vb, vbc], axis=1), p['lam'])
    out = merge_branches(oa, ob, ga, gb, p, lam_init)
    out_c = None
    if update_ctx:
        qac, qbc, gac, gbc = mixer_q(proj_c[..., KV_W:], p)
        oac = sink_attend(qac, kac, vac, p['sink_a']).reshape(hc.shape[0], hc.shape[1], A_WIDTH)
        obc = diff_attend(qbc, kbc, vbc, p['lam'])
        out_c = merge_branches(oac, obc, gac, gbc, p, lam_init)
    return out, out_c


def swiglu(x, wg, wu, wd):
    return (jax.nn.silu(x @ wg) * (x @ wu)) @ wd


def moe(tokens, p):
    T, D = tokens.shape
    scores = jax.nn.sigmoid((tokens @ p['w_router']).astype(jnp.float32))
    biased = scores + p['router_bias'].astype(jnp.float32)
    grp = biased.reshape(T, N_GROUPS, N_EXPERTS // N_GROUPS)
    grp_score = lax.top_k(grp, 2)[0].sum(-1)
    _, top_g = lax.top_k(grp_score, TOPK_GROUPS)
    gmask = jax.nn.one_hot(top_g, N_GROUPS, dtype=jnp.float32).sum(-2) > 0
    emask = jnp.repeat(gmask, N_EXPERTS // N_GROUPS, axis=1)
    _, idx = lax.top_k(jnp.where(emask, biased, -jnp.inf), TOP_K)
    w = jnp.take_along_axis(scores, idx, axis=-1)
    w = w / jnp.sum(w, axis=-1, keepdims=True) * ROUTED_SCALE

    M = T * TOP_K
    e_flat = idx.reshape(M)
    tok_flat = jnp.arange(M) // TOP_K
    w_flat = w.reshape(M)
    order = jnp.argsort(e_flat)
    e_s, tok_s, w_s = e_flat[order], tok_flat[order], w_flat[order]
    counts = jnp.bincount(e_flat, length=N_EXPERTS)
    start = jnp.cumsum(counts) - counts
    padded = (counts + MOE_BLOCK - 1) // MOE_BLOCK * MOE_BLOCK
    pend = jnp.cumsum(padded)
    pstart = pend - padded
    dest = pstart[e_s] + (jnp.arange(M) - start[e_s])
    nblk = -(-M // MOE_BLOCK) + N_EXPERTS
    P = nblk * MOE_BLOCK
    buf_tok = jnp.zeros((P,), jnp.int32).at[dest].set(tok_s.astype(jnp.int32))
    buf_w = jnp.zeros((P,), jnp.float32).at[dest].set(w_s)
    blk_expert = jnp.minimum(jnp.searchsorted(pend, jnp.arange(nblk) * MOE_BLOCK, side='right'),
                             N_EXPERTS - 1)

    def body(b, y):
        rows = lax.dynamic_slice_in_dim(buf_tok, b * MOE_BLOCK, MOE_BLOCK)
        wts = lax.dynamic_slice_in_dim(buf_w, b * MOE_BLOCK, MOE_BLOCK)
        e = blk_expert[b]
        out = swiglu(tokens[rows], p['w_gate_e'][e], p['w_up_e'][e], p['w_down_e'][e])
        return y.at[rows].add(out.astype(jnp.float32) * wts[:, None])

    y = lax.fori_loop(0, nblk, body, jnp.zeros((T, D), jnp.float32))
    shared = swiglu(tokens, p['w_gate_s'], p['w_up_s'], p['w_down_s']).astype(jnp.float32)
    return (y + shared).astype(tokens.dtype)


def setup_inputs(seed: int = 0) -> dict:
    key = jax.random.key(seed)
    ks = jax.random.split(key, 32)
    L, D = DEPTH, D_MODEL

    def nrm(k, shape, s):
        return jax.random.normal(k, shape, jnp.float32) * s

    return {
        "x": nrm(ks[0], (BATCH, SEQ, D), 1.0),
        "c": nrm(ks[1], (BATCH, D), 1.0),
        "ctx": nrm(ks[2], (BATCH, CTX_LEN, D), 1.0),
        "c_ctx": nrm(ks[3], (D,), 1.0),
        "w_ada": nrm(ks[4], (L, D, 6 * D), 0.5 * D ** -0.5),
        "b_ada": nrm(ks[5], (L, 6 * D), 0.02),
        "norm1_g": 1.0 + nrm(ks[6], (L, D), 0.1),
        "norm2_g": 1.0 + nrm(ks[7], (L, D), 0.1),
        "w_in": nrm(ks[8], (L, D, IN_W), D ** -0.5),
        "qnorm_a": 1.0 + nrm(ks[9], (L, HEAD_DIM), 0.1),
        "knorm_a": 1.0 + nrm(ks[10], (L, HEAD_DIM), 0.1),
        "sink_a": nrm(ks[11], (L, A_KV_HEADS, A_GROUP), 0.5),
        "qnorm_b": 1.0 + nrm(ks[12], (L, HEAD_DIM), 0.1),
        "knorm_b": 1.0 + nrm(ks[13], (L, HEAD_DIM), 0.1),
        "lam_q1": nrm(ks[14], (L, HEAD_DIM), 0.1),
        "lam_k1": nrm(ks[15], (L, HEAD_DIM), 0.1),
        "lam_q2": nrm(ks[16], (L, HEAD_DIM), 0.1),
        "lam_k2": nrm(ks[17], (L, HEAD_DIM), 0.1),
        "subln_g": 1.0 + nrm(ks[18], (L, B_V_DIM), 0.1),
        "w_pa": nrm(ks[19], (L, A_WIDTH, D), A_WIDTH ** -0.5),
        "w_pb": nrm(ks[20], (L, B_WIDTH, D), B_WIDTH ** -0.5),
        "w_o": nrm(ks[21], (L, D, D), D ** -0.5),
        "w_router": nrm(ks[22], (L, D, N_EXPERTS), D ** -0.5),
        "router_bias": nrm(ks[23], (L, N_EXPERTS), 0.01),
        "w_gate_e": nrm(ks[24], (L, N_EXPERTS, D, D_EXPERT), D ** -0.5),
        "w_up_e": nrm(ks[25], (L, N_EXPERTS, D, D_EXPERT), D ** -0.5),
        "w_down_e": nrm(ks[26], (L, N_EXPERTS, D_EXPERT, D), D_EXPERT ** -0.5),
        "w_gate_s": nrm(ks[27], (L, D, D_EXPERT), D ** -0.5),
        "w_up_s": nrm(ks[28], (L, D, D_EXPERT), D ** -0.5),
        "w_down_s": nrm(ks[29], (L, D_EXPERT, D), D_EXPERT ** -0.5),
    }


def reference(x, c, ctx, c_ctx, w_ada, b_ada, norm1_g, norm2_g, w_in, qnorm_a, knorm_a, sink_a,
              qnorm_b, knorm_b, lam_q1, lam_k1, lam_q2, lam_k2, subln_g, w_pa, w_pb, w_o,
              w_router, router_bias, w_gate_e, w_up_e, w_down_e, w_gate_s, w_up_s, w_down_s):
    B, n, D = x.shape
    cos, sin = rope_tables(n)
    for l in range(DEPTH):
        update_ctx = l < DEPTH - 1
        lam_init = 0.8 - 0.6 * math.exp(-0.3 * l)
        lam = (jnp.exp(jnp.sum(lam_q1[l].astype(jnp.float32) * lam_k1[l].astype(jnp.float32)))
               - jnp.exp(jnp.sum(lam_q2[l].astype(jnp.float32) * lam_k2[l].astype(jnp.float32)))
               + lam_init)
        p = {
            'w_in': w_in[l], 'qnorm_a': qnorm_a[l], 'knorm_a': knorm_a[l], 'sink_a': sink_a[l],
            'qnorm_b': qnorm_b[l], 'knorm_b': knorm_b[l], 'lam': lam, 'subln_g': subln_g[l],
            'w_pa': w_pa[l], 'w_pb': w_pb[l], 'w_o': w_o[l],
            'w_router': w_router[l], 'router_bias': router_bias[l],
            'w_gate_e': w_gate_e[l], 'w_up_e': w_up_e[l], 'w_down_e': w_down_e[l],
            'w_gate_s': w_gate_s[l], 'w_up_s': w_up_s[l], 'w_down_s': w_down_s[l],
        }
        mod = (jax.nn.silu(c) @ w_ada[l] + b_ada[l])[:, None, :]
        mod_c = jax.nn.silu(c_ctx) @ w_ada[l] + b_ada[l]
        sh1, sc1, g1, sh2, sc2, g2 = jnp.split(mod, 6, axis=-1)
        csh1, csc1, cg1, csh2, csc2, cg2 = jnp.split(mod_c, 6, axis=-1)

        h = rms_norm(x, norm1_g[l]) * (1.0 + sc1) + sh1
        hc = rms_norm(ctx, norm1_g[l]) * (1.0 + csc1) + csh1
        mix, mix_c = token_mixers(h, hc, cos, sin, p, lam_init, update_ctx)
        x = x + g1 * mix
        h2 = rms_norm(x, norm2_g[l]) * (1.0 + sc2) + sh2
        if update_ctx:
            ctx = ctx + cg1 * mix_c
            h2c = rms_norm(ctx, norm2_g[l]) * (1.0 + csc2) + csh2
            y = moe(jnp.concatenate([h2.reshape(-1, D), h2c.reshape(-1, D)], axis=0), p)
            x = x + g2 * y[:B * n].reshape(B, n, D)
            ctx = ctx + cg2 * y[B * n:].reshape(ctx.shape)
        else:
            x = x + g2 * moe(h2.reshape(-1, D), p).reshape(B, n, D)
    return x
```

```python
import contextlib
import math
import numpy as np
import concourse.bass as bass
import concourse.mybir as mybir
from concourse.bass_utils import run_bass_kernel_spmd

F32 = mybir.dt.float32
BF16 = mybir.dt.bfloat16
I32 = mybir.dt.int32
ALU = mybir.AluOpType
AF = mybir.ActivationFunctionType
AX = mybir.AxisListType

D = 1024
CTX = 256
NE = 256
EPS = 1e-6
LAM_INIT = 0.8 - 0.6 * math.exp(-0.3 * 0)


class Dep:
    __slots__ = ("w", "r")

    def __init__(self):
        self.w = None
        self.r = {}


class Buf:
    __slots__ = ("t", "d", "psum")

    def __init__(self, t, psum=False):
        self.t = t
        self.d = Dep()
        self.psum = psum


class Sched:
    def __init__(self, nc, es, ndma=20):
        self.nc = nc
        self.eng = {"pe": nc.tensor, "act": nc.scalar, "dve": nc.vector,
                    "pool": nc.gpsimd, "sp": nc.sync}
        self.sem = {}
        self.cnt = {}
        self.pending = {}
        self.seen = {k: {} for k in self.eng}
        for k in self.eng:
            self.sem[k] = es.enter_context(nc.semaphore("s_" + k))
            self.cnt[k] = 0
            self.pending[k] = False
        self.dq = {}
        for q in ("sp", "act", "pool"):
            sl = []
            for i in range(ndma):
                key = "d_%s_%d" % (q, i)
                self.sem[key] = es.enter_context(nc.semaphore(key))
                self.cnt[key] = 0
                sl.append(key)
            self.dq[q] = [sl, 0]
        self.ninst = 0

    def _wait(self, e, key, val):
        if val <= 0 or (e == "pe" and key == "pe"):
            return
        if self.seen[e].get(key, 0) >= val:
            return
        self.eng[e].wait_ge(self.sem[key], val)
        self.seen[e][key] = val

    def _deps(self, e, reads, writes):
        need = {}
        for b in reads:
            if b.w is not None:
                k, v = b.w
                if need.get(k, 0) < v:
                    need[k] = v
        for b in writes:
            if b.w is not None:
                k, v = b.w
                if need.get(k, 0) < v:
                    need[k] = v
            for k, v in b.r.items():
                if need.get(k, 0) < v:
                    need[k] = v
        for k, v in need.items():
            if k == e and v > self.cnt[e]:
                continue
            self._wait(e, k, v)

    def op(self, e, fn, reads=(), writes=(), inc=True):
        writes = list(writes) + [b for b in reads if isinstance(b, Buf) and b.psum and b not in writes]
        reads = [b for b in reads if not (isinstance(b, Buf) and b.psum)]
        reads = [b.d if isinstance(b, Buf) else b for b in reads]
        writes = [b.d if isinstance(b, Buf) else b for b in writes]
        self._deps(e, reads, writes)
        inst = fn(self.eng[e])
        self.ninst += 1
        val = self.cnt[e] + 1
        if inc:
            inst.then_inc(self.sem[e], 1)
            self.cnt[e] = val
            self.pending[e] = False
        else:
            self.pending[e] = True
        for b in reads:
            if b.r.get(e, 0) < val:
                b.r[e] = val
        for b in writes:
            b.w = (e, val)
            b.r = {}
        return inst

    def dma(self, q, fn, reads=(), writes=()):
        reads = [b.d if isinstance(b, Buf) else b for b in reads]
        writes = [b.d if isinstance(b, Buf) else b for b in writes]
        assert not self.pending[q]
        sl, i = self.dq[q]
        key = sl[i % len(sl)]
        self.dq[q][1] = i + 1
        self._wait(q, key, self.cnt[key])
        self._deps(q, reads, writes)
        inst = fn(self.eng[q])
        self.ninst += 1
        self.cnt[key] += 16
        val = self.cnt[key]
        inst.then_inc(self.sem[key], 16)
        for b in reads:
            if b.r.get(key, 0) < val:
                b.r[key] = val
        for b in writes:
            b.w = (key, val)
            b.r = {}
        return inst

    def barrier(self):
        for e in self.eng:
            assert not self.pending[e], e
        for e in self.eng:
            for key, v in self.cnt.items():
                if key != e:
                    self._wait(e, key, v)

    def finalize(self, e="sp"):
        for key, v in self.cnt.items():
            if key != e and v > 0:
                self._wait(e, key, v)


class Ring:
    def __init__(self, bufs):
        self.bufs = bufs
        self.i = 0

    def next(self):
        b = self.bufs[self.i % len(self.bufs)]
        self.i += 1
        return b


def build(S, debug=False, stopat=None):
    NT = S // 128
    NKT = NT + 2
    SK = S + CTX
    QC = min(512, S)
    NQC = S // QC
    NQS = QC // 128
    NBLK = (S * 8) // 128 + NE
    NB128 = (NBLK + 127) // 128
    assert NBLK % 16 == 0

    nc = bass.Bass("TRN2", target_bir_lowering=False)

    def din(name, shape, dt=F32):
        return nc.dram_tensor(name, list(shape), dt, kind="ExternalInput").ap()

    dbg_kind = "ExternalOutput"

    def dscr(name, shape, dt):
        return nc.dram_tensor(name, list(shape), dt, kind=dbg_kind).ap()

    x_d = din("x", [S, D])
    ctx_d = din("ctx", [CTX, D])
    cvec_d = din("cvec", [128, 8, 2])
    wada_d = din("w_ada", [D, 6 * D])
    bada_d = din("b_ada", [128, 48])
    n12_d = din("n12", [128, 2, 8])
    win_d = din("w_in", [D, 6400])
    gain_d = din("gain", [128, 3200])
    rope_d = din("rope", [SK, 2, 64])
    sink_d = din("sink", [128, 16])
    lamv_d = din("lamv", [128, 4, 64])
    subg_d = din("subg", [128, 128])
    wpa_d = din("w_pa", [D, D])
    wpb_d = din("w_pb", [D, D])
    wo_d = din("w_o", [D, D])
    wr_d = din("w_router", [D, NE])
    rb_d = din("rbias", [128, NE])
    _er = NE * 128 if stopat is None else 128
    wge_d = din("w_gate_e", [_er, 2048])
    wue_d = din("w_up_e", [_er, 2048])
    wde_d = din("w_down_e", [_er, 2048])
    wgs_d = din("w_gate_s", [D, 256])
    wus_d = din("w_up_s", [D, 256])
    wds_d = din("w_down_s", [256, D])
    cst_d = din("consts", [128, 5, 128])
    thr_d = din("thr", [128, NB128])
    out_d = nc.dram_tensor("out", [S, D], F32, kind="ExternalOutput").ap()

    mod_s = dscr("mod_s", [8, D], F32)
    kaT_s = dscr("kaT_s", [128, SK], BF16)
    va_s = dscr("va_s", [SK, 128], BF16)
    kbT_s = dscr("kbT_s", [D, SK], BF16)
    vb_s = dscr("vb_s", [SK, D], BF16)
    qaT_s = dscr("qaT_s", [D, S], BF16)
    qbT_s = dscr("qbT_s", [D, S], BF16)
    g_s = dscr("g_s", [S, 2 * D], BF16)
    oaT_s = dscr("oaT_s", [D, S], BF16)
    obT_s = dscr("obT_s", [D, S], BF16)
    x1_s = dscr("x1_s", [S, D], F32)
    h2_s = dscr("h2_s", [S, D], BF16)
    rank_s = dscr("rank_s", [S, NE], F32)
    wt_s = dscr("wt_s", [S, NE], F32)
    eb_s = dscr("eb_s", [1, NB128 * 128], F32)
    xg_s = dscr("xg_s", [NBLK * 128, D], BF16)
    ys_s = [dscr("ys%d_s" % i, [NBLK * 128, 512], F32) for i in range(2)]

    with contextlib.ExitStack() as es0:
        sc = Sched(nc, es0)
        op, dma = sc.op, sc.dma

        def mk(es, name, shape, dt):
            return Buf(es.enter_context(nc.sbuf_tensor("sb_" + name, list(shape), dt)))

        def mkring(es, name, shape, dt, n):
            return Ring([mk(es, "%s%d" % (name, i), shape, dt) for i in range(n)])

        def mkps(es, name, shape, dt):
            return Buf(es.enter_context(nc.psum_tensor("ps_" + name, list(shape), dt)), psum=True)

        def mkpsring(es, name, shape, dt, n):
            return Ring([mkps(es, "%s%d" % (name, i), shape, dt) for i in range(n)])

        cst = mk(es0, "cst", [128, 5, 128], BF16)
        dma("pool", lambda e: e.dma_start(out=cst.t[:], in_=cst_d), writes=[cst])
        ident = cst.t[:, 0, :]
        m_prev = cst.t[:, 1, :]
        m_next = cst.t[:, 2, :]
        tri = cst.t[:, 3, :]
        ones_bf = cst.t[:, 4, :]
        epsb = mk(es0, "epsb", [128, 1], F32)
        op("pool", lambda e: e.memset(epsb.t[:], EPS), writes=[epsb])
        idx8 = mk(es0, "idx8", [128, NT, 8], I32)
        w8 = mk(es0, "w8", [128, NT, 8], F32)
        idxw = mk(es0, "idxw", [128, NBLK], I32)
        _r1 = es0.enter_context(nc.gpsimd.register("bnd_rows"))
        nc.gpsimd.reg_mov(_r1, NBLK * 128 - 1)
        bnd_rows = nc.gpsimd.snap(_r1)
        _r2 = es0.enter_context(nc.gpsimd.register("bnd_w"))
        nc.gpsimd.reg_mov(_r2, NE * 128 - 1)
        bnd_w = nc.gpsimd.snap(_r2)

        def rsqrt_mean(dst, src, n, rd, wr):
            op("act", lambda e: e.activation(out=dst, in_=src, func=AF.Ln, scale=1.0 / n, bias=epsb.t[:, 0:1]),
               reads=rd + [epsb], writes=wr)
            op("act", lambda e: e.activation(out=dst, in_=dst, func=AF.Exp, scale=-0.5), reads=wr, writes=wr)

        with contextlib.ExitStack() as es:
            cv = mk(es, "cv", [128, 8, 2], F32)
            cs = mk(es, "cs", [128, 8, 2], F32)
            bada = mk(es, "bada", [128, 48], F32)
            n12 = mk(es, "n12", [128, 2, 8], F32)
            modv = mk(es, "modv", [128, 48, 2], F32)
            mv8 = mk(es, "mv8", [128, 8, 8], F32)
            war = mkring(es, "wa", [128, 8, 1024], F32, 2)
            pmod = mkps(es, "pmod", [128, 48, 2], F32)
            dma("sp", lambda e: e.dma_start(out=cv.t[:], in_=cvec_d), writes=[cv])
            dma("sp", lambda e: e.dma_start(out=bada.t[:], in_=bada_d), writes=[bada])
            dma("sp", lambda e: e.dma_start(out=n12.t[:], in_=n12_d), writes=[n12])
            op("act", lambda e: e.activation(out=cs.t[:], in_=cv.t[:], func=AF.Silu), reads=[cv], writes=[cs])
            for m6 in range(6):
                wa = war.next()
                dma("sp" if m6 % 2 == 0 else "act", lambda e: e.dma_start(
                    out=wa.t[:], in_=wada_d[:, m6 * 1024:(m6 + 1) * 1024].rearrange("(j p) n -> p j n", p=128)),
                    writes=[wa])
                for mm in range(8):
                    for j in range(8):
                        op("pe", lambda e: e.matmul(pmod.t[:, m6 * 8 + mm, :], lhsT=wa.t[:, j, mm * 128:(mm + 1) * 128],
                                                    rhs=cs.t[:, j, :], start=(j == 0), stop=(j == 7)),
                           reads=[wa, cs], writes=[pmod], inc=(j == 7))
            op("dve", lambda e: e.tensor_tensor(out=modv.t[:], in0=pmod.t[:],
                                                in1=bada.t[:].unsqueeze(2).to_broadcast([128, 48, 2]), op=ALU.add),
               reads=[pmod, bada], writes=[modv])
            def mvs(i):
                return mv8.t[:, :, i]
            def modc(ch, who):
                return modv.t[:, ch * 8:(ch + 1) * 8, who]
            def affine_gain(dst, scl, ng):
                op("dve", lambda e: e.scalar_tensor_tensor(out=dst, in0=scl, scalar=1.0, in1=ng, op0=ALU.add, op1=ALU.mult),
                   reads=[modv, n12], writes=[mv8])
            affine_gain(mvs(0), modc(1, 0), n12.t[:, 0, :])
            op("dve", lambda e: e.tensor_copy(out=mvs(1), in_=modc(0, 0)), reads=[modv], writes=[mv8])
            affine_gain(mvs(2), modc(1, 1), n12.t[:, 0, :])
            op("dve", lambda e: e.tensor_copy(out=mvs(3), in_=modc(0, 1)), reads=[modv], writes=[mv8])
            affine_gain(mvs(4), modc(4, 0), n12.t[:, 1, :])
            op("dve", lambda e: e.tensor_copy(out=mvs(5), in_=modc(3, 0)), reads=[modv], writes=[mv8])
            op("dve", lambda e: e.tensor_copy(out=mvs(6), in_=modc(2, 0)), reads=[modv], writes=[mv8])
            op("dve", lambda e: e.tensor_copy(out=mvs(7), in_=modc(5, 0)), reads=[modv], writes=[mv8])
            mod_dep = Dep()
            for i in range(8):
                dma("sp", lambda e: e.dma_start(out=mod_s[i].rearrange("(j p) -> p j", p=128), in_=mv8.t[:, :, i],
                                                allow_slow_non_contiguous=True),
                    reads=[mv8], writes=[mod_dep])
            sc.barrier()
        if stopat == 'A':
            sc.finalize("sp")
            return nc

        def load_bc(es, name, i, q="sp"):
            b = mk(es, name, [128, D], F32)
            dma(q, lambda e: e.dma_start(out=b.t[:], in_=mod_s[i:i + 1, :].partition_broadcast(128)),
                reads=[mod_dep], writes=[b])
            return b

        def norm_affine(xt, gbc, shbc, ssr, junkr, tmpr, hr):
            ss = ssr.next()
            junk = junkr.next()
            tmp = tmpr.next()
            h = hr.next()
            op("pool", lambda e: e.memset(ss.t[:], 0.0), writes=[ss])
            op("act", lambda e: e.activation(out=junk.t[:], in_=xt.t[:], func=AF.Square, accum_out=ss.t[:, 0:1]),
               reads=[xt, ss], writes=[junk, ss])
            rsqrt_mean(ss.t[:, 0:1], ss.t[:, 0:1], D, [ss], [ss])
            op("dve", lambda e: e.scalar_tensor_tensor(out=tmp.t[:], in0=xt.t[:], scalar=ss.t[:, 0:1], in1=gbc.t[:],
                                                       op0=ALU.mult, op1=ALU.mult), reads=[xt, ss, gbc], writes=[tmp])
            op("pool", lambda e: e.tensor_tensor(out=h.t[:], in0=tmp.t[:], in1=shbc.t[:], op=ALU.add),
               reads=[tmp, shbc], writes=[h])
            return h

        def transpose8(src, ptr, dstr, evac="act"):
            pt = ptr.next()
            dst = dstr.next()
            for j in range(8):
                op("pe", lambda e: e.transpose(out=pt.t[:, j, :], in_=src.t[:, j * 128:(j + 1) * 128], identity=ident),
                   reads=[src, cst], writes=[pt], inc=(j == 7))
            if evac == "act":
                op("act", lambda e: e.copy(out=dst.t[:], in_=pt.t[:]), reads=[pt], writes=[dst])
            else:
                op("dve", lambda e: e.tensor_copy(out=dst.t[:], in_=pt.t[:]), reads=[pt], writes=[dst])
            return dst

        with contextlib.ExitStack() as es:
            g1bc = load_bc(es, "g1bc", 0)
            sh1bc = load_bc(es, "sh1bc", 1, "act")
            g1cbc = load_bc(es, "g1cbc", 2)
            sh1cbc = load_bc(es, "sh1cbc", 3, "act")
            win = mk(es, "win", [128, 8, 6400], BF16)
            for j in range(8):
                for c4 in range(4):
                    dma("pool", lambda e: e.dma_start(out=win.t[:, j, c4 * 1600:(c4 + 1) * 1600],
                                                      in_=win_d[j * 128:(j + 1) * 128, c4 * 1600:(c4 + 1) * 1600]),
                        writes=[win])
            gain = mk(es, "gain", [128, 3200], F32)
            dma("sp", lambda e: e.dma_start(out=gain.t[:], in_=gain_d), writes=[gain])
            xr = mkring(es, "xt", [128, D], F32, 2)
            ssr = mkring(es, "ss", [128, 1], F32, 2)
            junkr = mkring(es, "junk", [128, D], BF16, 1)
            tmpr = mkring(es, "tmp", [128, D], F32, 1)
            hr = mkring(es, "h", [128, D], BF16, 2)
            hTr = mkring(es, "hT", [128, 8, 128], BF16, 2)
            roper = mkring(es, "rope", [128, 2, 64], F32, 2)
            sqr = mkring(es, "sq", [128, 512], F32, 2)
            ssgr = mkring(es, "ssg", [128, 8], F32, 2)
            y32r = mkring(es, "y32", [128, 512], F32, 2)
            t1r = mkring(es, "t1", [128, 512], F32, 2)
            t2r = mkring(es, "t2", [128, 512], F32, 2)
            qkbr = mkring(es, "qkb", [128, 512], BF16, 2)
            stgr = mkring(es, "stg", [128, 4, 128], BF16, 3)
            vstr = mkring(es, "vst", [128, 512], BF16, 3)
            ptr8 = mkpsring(es, "ptr8", [128, 8, 128], BF16, 1)
            ptr4 = mkpsring(es, "ptr4", [128, 4, 128], BF16, 1)
            ppr = mkpsring(es, "pp", [128, 512], F32, 5)
            scr_dep = {k: Dep() for k in ("kaT", "kbT", "vb", "qaT", "qbT", "g")}
            scr_dep_va = Dep()

            segs = [(0, 256, "kava", 0, None, 0),
                    (256, 512, "qk", 128, "kbT", 0), (768, 512, "qk", 640, "kbT", 512),
                    (1280, 512, "v", 0, "vb", 0), (1792, 512, "v", 0, "vb", 512),
                    (2304, 512, "qk", 1152, "qaT", 0), (2816, 512, "qk", 1664, "qaT", 512),
                    (3328, 512, "qk", 2176, "qbT", 0), (3840, 512, "qk", 2688, "qbT", 512),
                    (4352, 512, "g", 0, "g", 0), (4864, 512, "g", 0, "g", 512),
                    (5376, 512, "g", 0, "g", 1024), (5888, 512, "g", 0, "g", 1536)]
            scr_ap = {"kbT": kbT_s, "qaT": qaT_s, "qbT": qbT_s}
            dq = ["sp", "act"]
            dqi = [0]

            def nextq():
                dqi[0] += 1
                return dq[dqi[0] % 2]

            def qk_post(pp, w, gcol, rope, t, which, row0):
                G = w // 64
                sq = sqr.next(); ssg = ssgr.next(); y32 = y32r.next(); t1 = t1r.next(); t2 = t2r.next(); qkb = qkbr.next()
                op("act", lambda e: e.activation(out=sq.t[:, 0:w], in_=pp.t[:, 0:w], func=AF.Square), reads=[pp], writes=[sq])
                op("dve", lambda e: e.tensor_tensor(out=y32.t[:, 0:w], in0=pp.t[:, 0:w], in1=gain.t[:, gcol:gcol + w], op=ALU.mult),
                   reads=[pp, gain], writes=[y32])
                op("dve", lambda e: e.tensor_reduce(out=ssg.t[:, 0:G], in_=sq.t[:, 0:w].rearrange("p (g d) -> p g d", d=64),
                                                    axis=AX.X, op=ALU.add), reads=[sq], writes=[ssg])
                rsqrt_mean(ssg.t[:, 0:G], ssg.t[:, 0:G], 64, [ssg], [ssg])
                if stopat == "Q1":
                    return
                yv = y32.t[:, 0:w].rearrange("p (g d) -> p g d", d=64)
                op("dve", lambda e: e.tensor_tensor(out=t1.t[:, 0:w].rearrange("p (g d) -> p g d", d=64), in0=yv,
                                                    in1=rope.t[:, 0, :].unsqueeze(1).to_broadcast([128, G, 64]), op=ALU.mult),
                   reads=[y32, rope], writes=[t1])
                if stopat == "Q2":
                    return
                y5 = y32.t[:, 0:w].rearrange("p (g a b f) -> p (g a) b f", a=2, b=2, f=16)
                t5 = t2.t[:, 0:w].rearrange("p (g a b f) -> p (g a) b f", a=2, b=2, f=16)
                s5 = rope.t[:, 1, :].rearrange("p (a b f) -> p a b f", a=2, b=2, f=16)
                for a in range(2):
                    ya = y32.t[:, 0:w].rearrange("p (g a b f) -> p g a b f", a=2, b=2, f=16)[:, :, a]
                    ta = t2.t[:, 0:w].rearrange("p (g a b f) -> p g a b f", a=2, b=2, f=16)[:, :, a]
                    for b in range(2):
                        op("pool", lambda e: e.tensor_tensor(out=ta[:, :, b, :], in0=ya[:, :, 1 - b, :],
                                                             in1=s5[:, a, b, :].unsqueeze(1).to_broadcast([128, G, 16]), op=ALU.mult),
                           reads=[y32, rope], writes=[t2])
                op("pool", lambda e: e.tensor_tensor(out=t1.t[:, 0:w], in0=t1.t[:, 0:w], in1=t2.t[:, 0:w], op=ALU.add),
                   reads=[t1, t2], writes=[t1])
                if stopat == "Q3":
                    return
                op("dve", lambda e: e.tensor_tensor(out=qkb.t[:, 0:w].rearrange("p (g d) -> p g d", d=64),
                                                    in0=t1.t[:, 0:w].rearrange("p (g d) -> p g d", d=64),
                                                    in1=ssg.t[:, 0:G].unsqueeze(2).to_broadcast([128, G, 64]), op=ALU.mult),
                   reads=[t1, ssg], writes=[qkb])
                if stopat == "Q4":
                    return
                nb = w // 128
                pt = ptr4.next(); stg = stgr.next()
                for j in range(nb):
                    op("pe", lambda e: e.transpose(out=pt.t[:, j, :], in_=qkb.t[:, j * 128:(j + 1) * 128], identity=ident),
                       reads=[qkb, cst], writes=[pt], inc=(j == nb - 1))
                op("act", lambda e: e.copy(out=stg.t[:, 0:nb, :], in_=pt.t[:, 0:nb, :]), reads=[pt], writes=[stg])
                if which == "kaT":
                    dma(nextq(), lambda e: e.dma_start(out=kaT_s[:, t * 128:(t + 1) * 128], in_=stg.t[:, 0, :]),
                        reads=[stg], writes=[scr_dep["kaT"]])
                else:
                    dst = scr_ap[which][row0:row0 + w, t * 128:(t + 1) * 128].rearrange("(j p) n -> p j n", p=128)
                    dma(nextq(), lambda e: e.dma_start(out=dst, in_=stg.t[:, 0:nb, :]), reads=[stg], writes=[scr_dep[which]])

            if stopat == "B0":
                sc.barrier(); sc.finalize("sp"); return nc
            for t in range(NKT):
                is_ctx = t >= NT
                xt = xr.next()
                src = ctx_d[(t - NT) * 128:(t - NT + 1) * 128, :] if is_ctx else x_d[t * 128:(t + 1) * 128, :]
                dma("sp", lambda e: e.dma_start(out=xt.t[:], in_=src), writes=[xt])
                rope = roper.next()
                dma("act", lambda e: e.dma_start(out=rope.t[:], in_=rope_d[t * 128:(t + 1) * 128]), writes=[rope])
                h = norm_affine(xt, g1cbc if is_ctx else g1bc, sh1cbc if is_ctx else sh1bc, ssr, junkr, tmpr, hr)
                hT = transpose8(h, ptr8, hTr)
                for (c0, w, kind, gcol, which, row0) in segs:
                    if is_ctx and c0 >= 2304:
                        continue
                    if stopat == "B1" or (stopat == "B2" and kind in ("kava", "qk")):
                        continue
                    pp = ppr.next()
                    for j in range(8):
                        op("pe", lambda e: e.matmul(pp.t[:, 0:w], lhsT=hT.t[:, j, :], rhs=win.t[:, j, c0:c0 + w],
                                                    start=(j == 0), stop=(j == 7)), reads=[hT, win], writes=[pp], inc=(j == 7))
                    if kind == "kava":
                        qk_post(pp, 128, 0, rope, t, "kaT", 0)
                        vst = vstr.next()
                        op("act", lambda e: e.copy(out=vst.t[:, 0:128], in_=pp.t[:, 128:256]), reads=[pp], writes=[vst])
                        dma(nextq(), lambda e: e.dma_start(out=va_s[t * 128:(t + 1) * 128, :], in_=vst.t[:, 0:128]),
                            reads=[vst], writes=[scr_dep_va])
                    elif kind == "qk":
                        qk_post(pp, w, gcol, rope, t, which, row0)
                    elif kind == "v":
                        vst = vstr.next()
                        op("act", lambda e: e.copy(out=vst.t[:], in_=pp.t[:]), reads=[pp], writes=[vst])
                        dma(nextq(), lambda e: e.dma_start(out=vb_s[t * 128:(t + 1) * 128, row0:row0 + 512], in_=vst.t[:]),
                            reads=[vst], writes=[scr_dep["vb"]])
                    else:
                        vst = vstr.next()
                        op("act", lambda e: e.activation(out=vst.t[:], in_=pp.t[:], func=AF.Sigmoid), reads=[pp], writes=[vst])
                        dma(nextq(), lambda e: e.dma_start(out=g_s[t * 128:(t + 1) * 128, row0:row0 + 512], in_=vst.t[:]),
                            reads=[vst], writes=[scr_dep["g"]])
            sc.barrier()
        if stopat in ("B", "B1", "B2", "Q1", "Q2", "Q3", "Q4"):
            sc.finalize("sp")
            return nc

        with contextlib.ExitStack() as es:
            kaT = mk(es, "kaT", [64, 2, SK], BF16)
            dma("sp", lambda e: e.dma_start(out=kaT.t[:], in_=kaT_s.rearrange("(g d) n -> d g n", d=64)), writes=[kaT])
            va = mk(es, "va", [128, NKT, 2, 65], BF16)
            op("pool", lambda e: e.memset(va.t[:], 1.0), writes=[va])
            for g_ in range(2):
                for t0 in range(0, NKT, 8):
                    t1_ = min(NKT, t0 + 8)
                    dma("act" if g_ == 0 else "sp", lambda e: e.dma_start(
                        out=va.t[:, t0:t1_, g_, 0:64],
                        in_=va_s[t0 * 128:t1_ * 128, g_ * 64:(g_ + 1) * 64].rearrange("(t p) e -> p t e", p=128)), writes=[va])
            esink = mk(es, "esink", [128, 16], F32)
            dma("sp", lambda e: e.dma_start(out=esink.t[:], in_=sink_d), writes=[esink])
            op("act", lambda e: e.activation(out=esink.t[:], in_=esink.t[:], func=AF.Exp), reads=[esink], writes=[esink])
            qar = mkring(es, "qa", [64, 8, 128], BF16, 2)
            pTr = mkring(es, "pTa", [128, 1024], BF16, 2)
            psr = mkpsring(es, "psa", [128, 1024], F32, 2)
            po = [mkps(es, "poa%d" % i, [128, 512], F32) for i in range(2)]
            ptr8 = mkpsring(es, "ptr8d", [128, 8, 128], BF16, 1)
            oatr = mkring(es, "oatok", [128, D], BF16, 2)
            zr = mkring(es, "zz", [128, 16], F32, 2)
            oaTr = mkring(es, "oaT", [128, 8, 128], BF16, 2)
            oaT_dep = Dep()
            for i in range(NT):
                oat = oatr.next(); zz = zr.next()
                for g in range(2):
                    qa = qar.next()
                    dma("sp" if g == 0 else "act", lambda e: e.dma_start(
                        out=qa.t[:], in_=qaT_s[g * 512:(g + 1) * 512, i * 128:(i + 1) * 128].rearrange("(h d) n -> d h n", d=64)),
                        writes=[qa])
                    kts = [(t, t - i) for t in (i - 1, i, i + 1) if 0 <= t < NT] + [(NT, 0), (NT + 1, 0)]
                    for ki, (kt, rel) in enumerate(kts):
                        ps = psr.next(); pT = pTr.next()
                        for hf in range(2):
                            op("pe", lambda e: e.matmul(ps.t[:, hf * 512:(hf + 1) * 512], lhsT=kaT.t[:, g, kt * 128:(kt + 1) * 128],
                                                        rhs=qa.t[:, hf * 4:(hf + 1) * 4, :], start=True, stop=True),
                               reads=[kaT, qa], writes=[ps], inc=(hf == 1))
                        op("act", lambda e: e.activation(out=pT.t[:], in_=ps.t[:], func=AF.Exp, scale=0.125), reads=[ps], writes=[pT])
                        if rel != 0:
                            msk = m_prev if rel < 0 else m_next
                            pv3 = pT.t[:].rearrange("p (h q) -> p h q", q=128)
                            op("pool", lambda e: e.tensor_tensor(out=pv3, in0=pv3, in1=msk.unsqueeze(1).to_broadcast([128, 8, 128]),
                                                                 op=ALU.mult), reads=[pT, cst], writes=[pT])
                        for hh in range(8):
                            pob = po[hh // 4]
                            c0 = (hh % 4) * 65
                            op("pe", lambda e: e.matmul(pob.t[:, c0:c0 + 65], lhsT=pT.t[:, hh * 128:(hh + 1) * 128], rhs=va.t[:, kt, g, :],
                                                        start=(ki == 0 and hh % 4 == 0), stop=(ki == len(kts) - 1), skip_group_check=True),
                               reads=[pT, va], writes=[pob], inc=(hh % 4 == 3))
                    for half in range(2):
                        pv = po[half].t[:, 0:260].rearrange("p (h e) -> p h e", e=65)
                        h0 = g * 8 + half * 4
                        op("dve", lambda e: e.tensor_tensor(out=zz.t[:, h0:h0 + 4], in0=pv[:, :, 64], in1=esink.t[:, h0:h0 + 4], op=ALU.add),
                           reads=[po[half], esink], writes=[zz])
                        op("dve", lambda e: e.reciprocal(out=zz.t[:, h0:h0 + 4], in_=zz.t[:, h0:h0 + 4]), reads=[zz], writes=[zz])
                        op("dve", lambda e: e.tensor_tensor(out=oat.t[:, h0 * 64:(h0 + 4) * 64].rearrange("p (h e) -> p h e", e=64),
                                                            in0=pv[:, :, 0:64], in1=zz.t[:, h0:h0 + 4].unsqueeze(2).to_broadcast([128, 4, 64]),
                                                            op=ALU.mult), reads=[po[half], zz], writes=[oat])
                oaT = transpose8(oat, ptr8, oaTr)
                dma("sp", lambda e: e.dma_start(out=oaT_s[:, i * 128:(i + 1) * 128].rearrange("(j p) n -> p j n", p=128), in_=oaT.t[:]),
                    reads=[oaT], writes=[oaT_dep])
            sc.barrier()
        if stopat == 'D':
            sc.finalize("sp")
            return nc

        with contextlib.ExitStack() as es:
            zt = mk(es, "zt", [128, 4096], BF16)
            op("pool", lambda e: e.memset(zt.t[:], 0.0), writes=[zt])
            xg_dep = Dep()
            xgz = xg_s.rearrange("(a p r) n -> a p (r n)", p=128, r=4)
            for a in range(NBLK // 4):
                dma("pool", lambda e: e.dma_start(out=xgz[a], in_=zt.t[:]), reads=[zt], writes=[xg_dep])
            lamv = mk(es, "lamv", [128, 4, 64], F32)
            lam = mk(es, "lam", [128, 4], F32)
            ljunk = mk(es, "ljunk", [128, 64], F32)
            dma("sp", lambda e: e.dma_start(out=lamv.t[:], in_=lamv_d), writes=[lamv])
            op("pool", lambda e: e.memset(lam.t[:], 0.0), writes=[lam])
            for i2 in range(2):
                op("dve", lambda e: e.tensor_tensor(out=ljunk.t[:], in0=lamv.t[:, 2 * i2, :], in1=lamv.t[:, 2 * i2 + 1, :], op=ALU.mult),
                   reads=[lamv], writes=[ljunk])
                op("dve", lambda e: e.tensor_reduce(out=lam.t[:, i2:i2 + 1], in_=ljunk.t[:], axis=AX.X, op=ALU.add),
                   reads=[ljunk], writes=[lam])
            op("act", lambda e: e.activation(out=lam.t[:, 0:2], in_=lam.t[:, 0:2], func=AF.Exp), reads=[lam], writes=[lam])
            op("dve", lambda e: e.tensor_tensor(out=lam.t[:, 2:3], in0=lam.t[:, 1:2], in1=lam.t[:, 0:1], op=ALU.subtract), reads=[lam], writes=[lam])
            op("dve", lambda e: e.tensor_scalar(out=lam.t[:, 2:3], in0=lam.t[:, 2:3], scalar1=-LAM_INIT, scalar2=None, op0=ALU.add),
               reads=[lam], writes=[lam])
            subg = mk(es, "subg", [128, 128], F32)
            dma("sp", lambda e: e.dma_start(out=subg.t[:], in_=subg_d), writes=[subg])
            op("dve", lambda e: e.tensor_scalar(out=subg.t[:], in0=subg.t[:], scalar1=1.0 - LAM_INIT, scalar2=None, op0=ALU.mult),
               reads=[subg], writes=[subg])
            kbr = mkring(es, "kb", [64, 2, SK], BF16, 2)
            vbr = mkring(es, "vb", [128, NKT, 129], BF16, 2)
            for b_ in vbr.bufs:
                op("pool", lambda e: e.memset(b_.t[:], 1.0), writes=[b_])
            qbr = mkring(es, "qb", [64, 2, QC], BF16, 2)
            pTr = mkring(es, "pTe", [128, 2, QC], BF16, 2)
            psr = mkpsring(es, "pse", [128, 2, 512], F32, 2)
            accb = [mkps(es, "acc%d" % i, [128, 512], F32) for i in range(3)]
            ptb = mkps(es, "ptb", [128, 8, 128], BF16)
            rzr = mkring(es, "rz", [128, 2], F32, 2)
            tbr = mkring(es, "tb", [128, 128], F32, 2)
            dbr = mkring(es, "db", [128, 128], F32, 2)
            ssr = mkring(es, "sse", [128, 1], F32, 2)
            sjr = mkring(es, "sje", [128, 128], BF16, 1)
            obr = mkring(es, "obb", [128, 128], BF16, 2)
            ostr = mkring(es, "obst", [128, QC], BF16, 2)
            obT_dep = Dep()

            def accv(n):
                return accb[n // 3], accb[n // 3].t[:, (n % 3) * 129:(n % 3) * 129 + 129]

            for h in range(8):
                kb = kbr.next(); vb = vbr.next()
                dma("sp", lambda e: e.dma_start(out=kb.t[:], in_=kbT_s[h * 128:(h + 1) * 128, :].rearrange("(j d) n -> d j n", d=64)),
                    writes=[kb])
                for t0 in range(0, NKT, 8):
                    t1_ = min(NKT, t0 + 8)
                    dma("act", lambda e: e.dma_start(
                        out=vb.t[:, t0:t1_, 0:128],
                        in_=vb_s[t0 * 128:t1_ * 128, h * 128:(h + 1) * 128].rearrange("(t p) e -> p t e", p=128)), writes=[vb])
                for qc in range(NQC):
                    qb = qbr.next()
                    dma("sp", lambda e: e.dma_start(
                        out=qb.t[:], in_=qbT_s[h * 128:(h + 1) * 128, qc * QC:(qc + 1) * QC].rearrange("(j d) n -> d j n", d=64)),
                        writes=[qb])
                    for kt in range(NKT):
                        ps = psr.next(); pT = pTr.next()
                        for j in range(2):
                            op("pe", lambda e: e.matmul(ps.t[:, j, 0:QC], lhsT=kb.t[:, j, kt * 128:(kt + 1) * 128], rhs=qb.t[:, j, :],
                                                        start=True, stop=True), reads=[kb, qb], writes=[ps], inc=(j == 1))
                        op("act", lambda e: e.activation(out=pT.t[:], in_=ps.t[:, :, 0:QC], func=AF.Exp, scale=0.125),
                           reads=[ps], writes=[pT])
                        for j in range(2):
                            for qs in range(NQS):
                                ab_, av = accv(j * NQS + qs)
                                last = (j == 1 and qs == NQS - 1)
                                op("pe", lambda e: e.matmul(av, lhsT=pT.t[:, j, qs * 128:(qs + 1) * 128], rhs=vb.t[:, kt, :],
                                                            start=(kt == 0 and (j * NQS + qs) % 3 == 0), stop=(kt == NKT - 1), skip_group_check=True),
                                   reads=[pT, vb], writes=[ab_], inc=last)
                    ost = ostr.next()
                    for qs in range(NQS):
                        b0, a0 = accv(qs)
                        b1, a1 = accv(NQS + qs)
                        rz = rzr.next(); tb = tbr.next(); db = dbr.next(); ss = ssr.next(); sj = sjr.next(); ob = obr.next()
                        op("dve", lambda e: e.reciprocal(out=rz.t[:, 0:1], in_=a0[:, 128:129]), reads=[b0], writes=[rz])
                        op("dve", lambda e: e.reciprocal(out=rz.t[:, 1:2], in_=a1[:, 128:129]), reads=[b1], writes=[rz])
                        op("dve", lambda e: e.tensor_tensor(out=rz.t[:, 1:2], in0=rz.t[:, 1:2], in1=lam.t[:, 2:3], op=ALU.mult),
                           reads=[rz, lam], writes=[rz])
                        op("dve", lambda e: e.tensor_scalar(out=tb.t[:], in0=a1[:, 0:128], scalar1=rz.t[:, 1:2], scalar2=None, op0=ALU.mult),
                           reads=[b1, rz], writes=[tb])
                        op("dve", lambda e: e.scalar_tensor_tensor(out=db.t[:], in0=a0[:, 0:128], scalar=rz.t[:, 0:1], in1=tb.t[:],
                                                                   op0=ALU.mult, op1=ALU.add), reads=[b0, rz, tb], writes=[db])
                        op("pool", lambda e: e.memset(ss.t[:], 0.0), writes=[ss])
                        op("act", lambda e: e.activation(out=sj.t[:], in_=db.t[:], func=AF.Square, accum_out=ss.t[:, 0:1]),
                           reads=[db, ss], writes=[sj, ss])
                        rsqrt_mean(ss.t[:, 0:1], ss.t[:, 0:1], 128, [ss], [ss])
                        op("dve", lambda e: e.scalar_tensor_tensor(out=ob.t[:], in0=db.t[:], scalar=ss.t[:, 0:1], in1=subg.t[:],
                                                                   op0=ALU.mult, op1=ALU.mult), reads=[db, ss, subg], writes=[ob])
                        op("pe", lambda e: e.transpose(out=ptb.t[:, 0, :], in_=ob.t[:], identity=ident), reads=[ob, cst], writes=[ptb])
                        op("act", lambda e: e.copy(out=ost.t[:, qs * 128:(qs + 1) * 128], in_=ptb.t[:, 0, :]), reads=[ptb], writes=[ost])
                    dma("act", lambda e: e.dma_start(out=obT_s[h * 128:(h + 1) * 128, qc * QC:(qc + 1) * QC], in_=ost.t[:]),
                        reads=[ost], writes=[obT_dep])
            sc.barrier()
        if stopat == 'E':
            sc.finalize("sp")
            return nc

        base = mk(es0, "base", [128, NE], F32)
        op("pool", lambda e: e.memset(base.t[:], 0.0), writes=[base])
        with contextlib.ExitStack() as es:
            def loadw(name, src_d, kch, ncol, q="pool"):
                wt = mk(es, name, [128, kch, ncol], BF16)
                for j in range(kch):
                    dma(q, lambda e: e.dma_start(out=wt.t[:, j, :], in_=src_d[j * 128:(j + 1) * 128, :]), writes=[wt])
                return wt
            wpa = loadw("wpa", wpa_d, 8, D)
            wpb = loadw("wpb", wpb_d, 8, D)
            wo = loadw("wo", wo_d, 8, D)
            wr = loadw("wr", wr_d, 8, NE)
            wds = loadw("wds", wds_d, 2, D)
            wgus = mk(es, "wgus", [128, 8, 512], BF16)
            for j in range(8):
                dma("pool", lambda e: e.dma_start(out=wgus.t[:, j, 0:256], in_=wgs_d[j * 128:(j + 1) * 128, :]), writes=[wgus])
                dma("pool", lambda e: e.dma_start(out=wgus.t[:, j, 256:512], in_=wus_d[j * 128:(j + 1) * 128, :]), writes=[wgus])
            g2bc = load_bc(es, "g2bc", 4)
            sh2bc = load_bc(es, "sh2bc", 5, "act")
            gate1 = load_bc(es, "gate1", 6)
            gate2 = load_bc(es, "gate2", 7, "act")
            rb = mk(es, "rb", [128, NE], F32)
            dma("sp", lambda e: e.dma_start(out=rb.t[:], in_=rb_d), writes=[rb])
            oaTr = mkring(es, "oaTf", [128, 8, 128], BF16, 2)
            obTr = mkring(es, "obTf", [128, 8, 128], BF16, 2)
            gtr = mkring(es, "gt", [128, 2 * D], BF16, 2)
            xr = mkring(es, "xf", [128, D], F32, 2)
            t1r = mkring(es, "t1f", [128, D], F32, 2)
            t2r = mkring(es, "t2f", [128, D], F32, 1)
            zr = mkring(es, "zf", [128, D], BF16, 2)
            zTr = mkring(es, "zTf", [128, 8, 128], BF16, 2)
            x1r = mkring(es, "x1f", [128, D], F32, 2)
            x1sr = mkring(es, "x1sf", [128, D], F32, 2)
            ssr = mkring(es, "ssf", [128, 1], F32, 2)
            junkr = mkring(es, "junkf", [128, D], BF16, 1)
            tmpr = mkring(es, "tmpf", [128, D], F32, 1)
            h2r = mkring(es, "h2f", [128, D], BF16, 2)
            h2Tr = mkring(es, "h2Tf", [128, 8, 128], BF16, 2)
            sgr = mkring(es, "sgf", [128, 256], F32, 2)
            abr = mkring(es, "abf", [128, 256], BF16, 2)
            aTr = mkring(es, "aTf", [128, 2, 128], BF16, 2)
            r256 = mkring(es, "r256", [128, NE], F32, 8)
            selbr = mkring(es, "selb", [128, NE], BF16, 2)
            r8 = mkring(es, "r8", [128, 8], F32, 8)
            big = mkpsring(es, "bigf", [128, D], F32, 2)
            ptr8 = mkpsring(es, "ptr8f", [128, 8, 128], BF16, 1)
            sm1 = mkps(es, "sm1", [128, 512], F32)
            sm2 = mkps(es, "sm2", [128, 512], F32)
            hid = mkps(es, "hidf", [128, 512], F32)
            x1_dep = Dep(); h2_dep = Dep(); rank_dep = Dep(); wt_dep = Dep()

            def mm8(pdst, lhsT, w, ncols, kch=8):
                for n in range(ncols // 512):
                    for j in range(kch):
                        op("pe", lambda e: e.matmul(pdst.t[:, n * 512:(n + 1) * 512], lhsT=lhsT.t[:, j, :], rhs=w.t[:, j, n * 512:(n + 1) * 512],
                                                    start=(j == 0), stop=(j == kch - 1)), reads=[lhsT, w], writes=[pdst],
                           inc=(j == kch - 1 and n == ncols // 512 - 1))

            for t in range(NT):
                rows = slice(t * 128, (t + 1) * 128)
                oaT = oaTr.next(); obT = obTr.next(); gt = gtr.next(); xt = xr.next()
                dma("sp", lambda e: e.dma_start(out=oaT.t[:], in_=oaT_s[:, rows].rearrange("(j p) n -> p j n", p=128)), writes=[oaT])
                dma("act", lambda e: e.dma_start(out=obT.t[:], in_=obT_s[:, rows].rearrange("(j p) n -> p j n", p=128)), writes=[obT])
                dma("sp", lambda e: e.dma_start(out=gt.t[:], in_=g_s[rows, :]), writes=[gt])
                dma("act", lambda e: e.dma_start(out=xt.t[:], in_=x_d[rows, :]), writes=[xt])
                pya = big.next(); mm8(pya, oaT, wpa, D)
                pyb = big.next(); mm8(pyb, obT, wpb, D)
                t1 = t1r.next(); t2 = t2r.next(); z = zr.next()
                op("dve", lambda e: e.tensor_tensor(out=t1.t[:], in0=pya.t[:], in1=gt.t[:, 0:D], op=ALU.mult), reads=[pya, gt], writes=[t1])
                op("dve", lambda e: e.tensor_tensor(out=t2.t[:], in0=pyb.t[:], in1=gt.t[:, D:2 * D], op=ALU.mult), reads=[pyb, gt], writes=[t2])
                op("pool", lambda e: e.tensor_tensor(out=z.t[:], in0=t1.t[:], in1=t2.t[:], op=ALU.add), reads=[t1, t2], writes=[z])
                zT = transpose8(z, ptr8, zTr)
                pmx = big.next(); mm8(pmx, zT, wo, D)
                t1 = t1r.next(); x1 = x1r.next()
                op("dve", lambda e: e.tensor_tensor(out=t1.t[:], in0=pmx.t[:], in1=gate1.t[:], op=ALU.mult), reads=[pmx, gate1], writes=[t1])
                op("pool", lambda e: e.tensor_tensor(out=x1.t[:], in0=t1.t[:], in1=xt.t[:], op=ALU.add), reads=[t1, xt], writes=[x1])
                h2 = norm_affine(x1, g2bc, sh2bc, ssr, junkr, tmpr, h2r)
                dma("sp", lambda e: e.dma_start(out=h2_s[rows, :], in_=h2.t[:]), reads=[h2], writes=[h2_dep])
                h2T = transpose8(h2, ptr8, h2Tr)
                for j in range(8):
                    op("pe", lambda e: e.matmul(hid.t[:], lhsT=h2T.t[:, j, :], rhs=wgus.t[:, j, :], start=(j == 0), stop=(j == 7)),
                       reads=[h2T, wgus], writes=[hid], inc=(j == 7))
                sg = sgr.next(); ab = abr.next(); aT = aTr.next()
                op("act", lambda e: e.activation(out=sg.t[:], in_=hid.t[:, 0:256], func=AF.Silu), reads=[hid], writes=[sg])
                op("dve", lambda e: e.tensor_tensor(out=ab.t[:], in0=sg.t[:], in1=hid.t[:, 256:512], op=ALU.mult), reads=[sg, hid], writes=[ab])
                pt = ptr8.next()
                for j in range(2):
                    op("pe", lambda e: e.transpose(out=pt.t[:, j, :], in_=ab.t[:, j * 128:(j + 1) * 128], identity=ident),
                       reads=[ab, cst], writes=[pt], inc=(j == 1))
                op("act", lambda e: e.copy(out=aT.t[:], in_=pt.t[:, 0:2, :]), reads=[pt], writes=[aT])
                pys = big.next(); mm8(pys, aT, wds, D, kch=2)
                t1 = t1r.next(); x1s = x1sr.next()
                op("dve", lambda e: e.tensor_tensor(out=t1.t[:], in0=pys.t[:], in1=gate2.t[:], op=ALU.mult), reads=[pys, gate2], writes=[t1])
                op("pool", lambda e: e.tensor_tensor(out=x1s.t[:], in0=t1.t[:], in1=x1.t[:], op=ALU.add), reads=[t1, x1], writes=[x1s])
                dma("act", lambda e: e.dma_start(out=x1_s[rows, :], in_=x1s.t[:]), reads=[x1s], writes=[x1_dep])
                for j in range(8):
                    op("pe", lambda e: e.matmul(sm1.t[:, 0:256], lhsT=h2T.t[:, j, :], rhs=wr.t[:, j, :], start=(j == 0), stop=(j == 7)),
                       reads=[h2T, wr], writes=[sm1], inc=(j == 7))
                scs = r256.next(); bia = r256.next(); mb = r256.next(); sel = r256.next(); wv = r256.next(); rk = r256.next()
                gs = r8.next(); m8 = r8.next(); g8 = r8.next(); gm = r8.next(); tn = r8.next(); t8 = r8.next(); ws = r8.next()
                op("act", lambda e: e.activation(out=scs.t[:], in_=sm1.t[:, 0:256], func=AF.Sigmoid), reads=[sm1], writes=[scs])
                op("dve", lambda e: e.tensor_tensor(out=bia.t[:], in0=scs.t[:], in1=rb.t[:], op=ALU.add), reads=[scs, rb], writes=[bia])
                for gi in range(8):
                    op("dve", lambda e: e.max(out=m8.t[:], in_=bia.t[:, gi * 32:(gi + 1) * 32]), reads=[bia], writes=[m8])
                    op("dve", lambda e: e.tensor_tensor(out=gs.t[:, gi:gi + 1], in0=m8.t[:, 0:1], in1=m8.t[:, 1:2], op=ALU.add),
                       reads=[m8], writes=[gs])
                op("dve", lambda e: e.max(out=g8.t[:], in_=gs.t[:]), reads=[gs], writes=[g8])
                op("dve", lambda e: e.tensor_scalar(out=gm.t[:], in0=gs.t[:], scalar1=g8.t[:, 3:4], scalar2=None, op0=ALU.is_ge),
                   reads=[gs, g8], writes=[gm])
                op("dve", lambda e: e.tensor_scalar(out=tn.t[:], in0=gm.t[:], scalar1=4.0, scalar2=-4.0, op0=ALU.mult, op1=ALU.add),
                   reads=[gm], writes=[tn])
                b3 = bia.t[:].rearrange("p (g k) -> p g k", k=32)
                m3 = mb.t[:].rearrange("p (g k) -> p g k", k=32)
                op("dve", lambda e: e.tensor_tensor(out=m3, in0=b3, in1=gm.t[:].unsqueeze(2).to_broadcast([128, 8, 32]), op=ALU.mult),
                   reads=[bia, gm], writes=[mb])
                op("dve", lambda e: e.tensor_tensor(out=m3, in0=m3, in1=tn.t[:].unsqueeze(2).to_broadcast([128, 8, 32]), op=ALU.add),
                   reads=[mb, tn], writes=[mb])
                op("dve", lambda e: e.max(out=t8.t[:], in_=mb.t[:]), reads=[mb], writes=[t8])
                op("dve", lambda e: e.tensor_scalar(out=sel.t[:], in0=mb.t[:], scalar1=t8.t[:, 7:8], scalar2=None, op0=ALU.is_ge),
                   reads=[mb, t8], writes=[sel])
                op("dve", lambda e: e.tensor_tensor(out=wv.t[:], in0=scs.t[:], in1=sel.t[:], op=ALU.mult), reads=[scs, sel], writes=[wv])
                op("dve", lambda e: e.tensor_reduce(out=ws.t[:, 0:1], in_=wv.t[:], axis=AX.X, op=ALU.add), reads=[wv], writes=[ws])
                op("dve", lambda e: e.reciprocal(out=ws.t[:, 0:1], in_=ws.t[:, 0:1]), reads=[ws], writes=[ws])
                op("dve", lambda e: e.tensor_scalar(out=wv.t[:], in0=wv.t[:], scalar1=ws.t[:, 0:1], scalar2=2.5, op0=ALU.mult, op1=ALU.mult),
                   reads=[wv, ws], writes=[wv])
                dma("sp", lambda e: e.dma_start(out=wt_s[rows, :], in_=wv.t[:]), reads=[wv], writes=[wt_dep])
                selb = selbr.next()
                op("pool", lambda e: e.tensor_copy(out=selb.t[:], in_=sel.t[:]), reads=[sel], writes=[selb])
                op("pe", lambda e: e.matmul(sm1.t[:, 256:512], lhsT=tri, rhs=selb.t[:], start=True, stop=True), reads=[selb, cst], writes=[sm1], inc=False)
                op("pe", lambda e: e.matmul(sm2.t[:, 0:256], lhsT=ones_bf, rhs=selb.t[:], start=True, stop=True), reads=[selb, cst], writes=[sm2])
                op("dve", lambda e: e.tensor_tensor(out=rk.t[:], in0=sm1.t[:, 256:512], in1=base.t[:], op=ALU.add), reads=[sm1, base], writes=[rk])
                dma("act", lambda e: e.dma_start(out=rank_s[rows, :], in_=rk.t[:]), reads=[rk], writes=[rank_dep])
                op("dve", lambda e: e.tensor_tensor(out=base.t[:], in0=sm2.t[:, 0:256], in1=base.t[:], op=ALU.add), reads=[sm2, base], writes=[base])
            sc.barrier()
        if stopat == 'F':
            sc.finalize("sp")
            return nc

        with contextlib.ExitStack() as es:
            gate2 = load_bc(es, "gate2g", 7, "act")
            thr = mk(es, "thr", [128, NB128], F32)
            dma("sp", lambda e: e.dma_start(out=thr.t[:], in_=thr_d), writes=[thr])
            qm = mk(es, "qm", [128, NE], F32)
            pad = mk(es, "pad", [128, NE], F32)
            cum = [mk(es, "cum%d" % i, [128, NE], F32) for i in range(2)]
            pstart = mk(es, "pstart", [128, NE], F32)
            op("dve", lambda e: e.memset(qm.t[:], 0.0), writes=[qm])
            for m_ in range(NT):
                op("dve", lambda e: e.scalar_tensor_tensor(out=qm.t[:], in0=base.t[:], scalar=128.0 * m_, in1=qm.t[:],
                                                           op0=ALU.is_gt, op1=ALU.add), reads=[base, qm], writes=[qm])
            op("dve", lambda e: e.tensor_scalar(out=pad.t[:], in0=qm.t[:], scalar1=128.0, scalar2=None, op0=ALU.mult), reads=[qm], writes=[pad])
            cur = pad
            s_ = 1
            k_ = 0
            while s_ < NE:
                nxt = cum[k_ % 2]
                op("dve", lambda e: e.tensor_copy(out=nxt.t[:, 0:s_], in_=cur.t[:, 0:s_]), reads=[cur], writes=[nxt])
                op("dve", lambda e: e.tensor_tensor(out=nxt.t[:, s_:NE], in0=cur.t[:, s_:NE], in1=cur.t[:, 0:NE - s_], op=ALU.add),
                   reads=[cur], writes=[nxt])
                cur = nxt
                s_ *= 2
                k_ += 1
            pend = cur
            op("dve", lambda e: e.tensor_tensor(out=pstart.t[:], in0=pend.t[:], in1=pad.t[:], op=ALU.subtract), reads=[pend, pad], writes=[pstart])
            eb = mk(es, "eb", [128, NB128], F32)
            ebrep = mk(es, "ebrep", [128, NB128 * 128], F32)
            pcol = mk(es, "pcol", [128, 1], F32)
            for c in range(NB128):
                op("dve", lambda e: e.tensor_scalar(out=qm.t[:], in0=pend.t[:], scalar1=thr.t[:, c:c + 1], scalar2=None, op0=ALU.is_le),
                   reads=[pend, thr], writes=[qm])
                op("dve", lambda e: e.tensor_reduce(out=eb.t[:, c:c + 1], in_=qm.t[:], axis=AX.X, op=ALU.add), reads=[qm], writes=[eb])
            eb_dep = Dep()
            dma("sp", lambda e: e.dma_start(out=eb_s[0].rearrange("(c p) -> p c", p=128), in_=eb.t[:], allow_slow_non_contiguous=True),
                reads=[eb], writes=[eb_dep])
            dma("sp", lambda e: e.dma_start(out=ebrep.t[:], in_=eb_s.partition_broadcast(128)), reads=[eb_dep], writes=[ebrep])
            op("dve", lambda e: e.tensor_scalar(out=pcol.t[:], in0=thr.t[:, 0:1], scalar1=1.0 / 128, scalar2=None, op0=ALU.mult),
               reads=[thr], writes=[pcol])
            op("dve", lambda e: e.tensor_scalar(out=idxw.t[:], in0=ebrep.t[:, 0:NBLK], scalar1=128.0, scalar2=pcol.t[:, 0:1],
                                                op0=ALU.mult, op1=ALU.add), reads=[ebrep, pcol], writes=[idxw])
            op("pool", lambda e: e.memset(w8.t[:], 0.0), writes=[w8])
            rkr = mkring(es, "rkg", [128, NE], F32, 2)
            wvr = mkring(es, "wvg", [128, NE], F32, 2)
            h2r = mkring(es, "h2g", [128, D], BF16, 2)
            r256 = mkring(es, "r256g", [128, NE], F32, 4)
            d8r = mkring(es, "d8g", [128, 8], F32, 2)
            for t in range(NT):
                rows = slice(t * 128, (t + 1) * 128)
                rk = rkr.next(); wv = wvr.next(); h2 = h2r.next()
                dma("sp", lambda e: e.dma_start(out=rk.t[:], in_=rank_s[rows, :]), writes=[rk])
                dma("act", lambda e: e.dma_start(out=wv.t[:], in_=wt_s[rows, :]), writes=[wv])
                dma("sp", lambda e: e.dma_start(out=h2.t[:], in_=h2_s[rows, :]), writes=[h2])
                selm = r256.next(); dm = r256.next(); jk = r256.next(); d8 = d8r.next()
                op("dve", lambda e: e.tensor_scalar(out=selm.t[:], in0=wv.t[:], scalar1=0.0, scalar2=None, op0=ALU.is_gt), reads=[wv], writes=[selm])
                op("dve", lambda e: e.tensor_tensor(out=dm.t[:], in0=rk.t[:], in1=pstart.t[:], op=ALU.add), reads=[rk, pstart], writes=[dm])
                op("dve", lambda e: e.scalar_tensor_tensor(out=dm.t[:], in0=dm.t[:], scalar=1.0, in1=selm.t[:], op0=ALU.add, op1=ALU.mult),
                   reads=[dm, selm], writes=[dm])
                op("dve", lambda e: e.max(out=d8.t[:], in_=dm.t[:]), reads=[dm], writes=[d8])
                op("dve", lambda e: e.tensor_scalar(out=idx8.t[:, t, :], in0=d8.t[:], scalar1=-1.0, scalar2=None, op0=ALU.add),
                   reads=[d8], writes=[idx8])
                for k in range(8):
                    op("dve", lambda e: e.scalar_tensor_tensor(out=jk.t[:], in0=dm.t[:], scalar=d8.t[:, k:k + 1], in1=wv.t[:],
                                                               op0=ALU.is_equal, op1=ALU.mult, accum_out=w8.t[:, t, k:k + 1]),
                       reads=[dm, d8, wv, w8], writes=[jk, w8])
                for k in range(8):
                    dma("pool", lambda e: e.indirect_dma_start(
                        out=xg_s, out_offset=bass.IndirectOffsetOnAxis(ap=idx8.t[:, t, k:k + 1], axis=0), in_=h2.t[:], in_offset=None,
                        bounds_check=bnd_rows, oob_is_err=False), reads=[h2, idx8], writes=[xg_dep])
            xgr = mkring(es, "xgb", [128, D], BF16, 3)
            wgr = mkring(es, "wgb", [128, 8, 256], BF16, 2)
            wur = mkring(es, "wub", [128, 8, 256], BF16, 2)
            wdr = mkring(es, "wdb", [128, 2, D], BF16, 2)
            for rr in (wgr, wur, wdr):
                for b_ in rr.bufs:
                    op("pool", lambda e: e.memset(b_.t[:], 0.0), writes=[b_])
            xTr = mkring(es, "xTg", [128, 8, 128], BF16, 2)
            sgr = mkring(es, "sgg", [128, 256], F32, 2)
            abr = mkring(es, "abg", [128, 256], BF16, 2)
            aTr = mkring(es, "aTg", [128, 2, 128], BF16, 2)
            ystr = mkring(es, "ystg", [128, D], F32, 2)
            ptr8 = mkpsring(es, "ptr8g", [128, 8, 128], BF16, 1)
            ptr2 = mkpsring(es, "ptr2g", [128, 8, 128], BF16, 1)
            hidr = mkpsring(es, "hidg", [128, 512], F32, 2)
            pyr = mkpsring(es, "pyg", [128, D], F32, 2)
            ys_dep = Dep()
            import os as _os
            for b in range(min(NBLK, int(_os.environ.get('MAXBLK', '100000')))):
                xg = xgr.next(); wg = wgr.next(); wu = wur.next(); wd = wdr.next()
                dma("sp", lambda e: e.dma_start(out=xg.t[:], in_=xg_s[b * 128:(b + 1) * 128, :]), reads=[xg_dep], writes=[xg])
                for (wt_, src_) in ((wg, wge_d), (wu, wue_d), (wd, wde_d)):
                    dma("pool", lambda e: e.indirect_dma_start(
                        out=wt_.t[:].rearrange("p j n -> p (j n)"), out_offset=None, in_=src_,
                        in_offset=bass.IndirectOffsetOnAxis(ap=idxw.t[:, b:b + 1], axis=0),
                        bounds_check=bnd_w, oob_is_err=False), reads=[idxw], writes=[wt_])
                pt = ptr8.next(); xT = xTr.next()
                xv = xg.t[:].rearrange("p (q j) -> p j q", j=8)
                for j in range(8):
                    op("pe", lambda e: e.transpose(out=pt.t[:, j, :], in_=xv[:, j, :], identity=ident), reads=[xg, cst], writes=[pt], inc=(j == 7))
                op("act", lambda e: e.copy(out=xT.t[:], in_=pt.t[:]), reads=[pt], writes=[xT])
                hid = hidr.next()
                for j in range(8):
                    op("pe", lambda e: e.matmul(hid.t[:, 0:256], lhsT=xT.t[:, j, :], rhs=wg.t[:, j, :], start=(j == 0), stop=(j == 7), skip_group_check=True),
                       reads=[xT, wg], writes=[hid], inc=False)
                    op("pe", lambda e: e.matmul(hid.t[:, 256:512], lhsT=xT.t[:, j, :], rhs=wu.t[:, j, :], start=False, stop=(j == 7), skip_group_check=True),
                       reads=[xT, wu], writes=[hid], inc=(j == 7))
                sg = sgr.next(); ab = abr.next(); aT = aTr.next()
                op("act", lambda e: e.activation(out=sg.t[:], in_=hid.t[:, 0:256], func=AF.Silu), reads=[hid], writes=[sg])
                op("dve", lambda e: e.tensor_tensor(out=ab.t[:], in0=sg.t[:], in1=hid.t[:, 256:512], op=ALU.mult), reads=[sg, hid], writes=[ab])
                pt2 = ptr2.next()
                av = ab.t[:].rearrange("p (q j) -> p j q", j=2)
                for j in range(2):
                    op("pe", lambda e: e.transpose(out=pt2.t[:, j, :], in_=av[:, j, :], identity=ident), reads=[ab, cst], writes=[pt2], inc=(j == 1))
                op("dve", lambda e: e.tensor_copy(out=aT.t[:], in_=pt2.t[:, 0:2, :]), reads=[pt2], writes=[aT])
                py = pyr.next()
                for n in range(2):
                    for j in range(2):
                        op("pe", lambda e: e.matmul(py.t[:, n * 512:(n + 1) * 512], lhsT=aT.t[:, j, :], rhs=wd.t[:, j, n * 512:(n + 1) * 512],
                                                    start=(j == 0), stop=(j == 1)), reads=[aT, wd], writes=[py], inc=(j == 1 and n == 1))
                yst = ystr.next()
                if b % 2 == 0:
                    op("act", lambda e: e.copy(out=yst.t[:], in_=py.t[:]), reads=[py], writes=[yst])
                else:
                    op("dve", lambda e: e.tensor_copy(out=yst.t[:], in_=py.t[:]), reads=[py], writes=[yst])
                for hf in range(2):
                    dma("act" if hf == 0 else "sp", lambda e: e.dma_start(out=ys_s[hf][b * 128:(b + 1) * 128, :], in_=yst.t[:, hf * 512:(hf + 1) * 512]),
                        reads=[yst], writes=[ys_dep])
            gkr = mkring(es, "gkg", [128, D], F32, 3)
            for b_ in gkr.bufs:
                op("pool", lambda e: e.memset(b_.t[:], 0.0), writes=[b_])
            accr = mkring(es, "accg", [128, D], F32, 2)
            x1r = mkring(es, "x1g", [128, D], F32, 2)
            out_dep = Dep()
            for t in range(NT):
                rows = slice(t * 128, (t + 1) * 128)
                x1s = x1r.next(); acc = accr.next()
                dma("sp", lambda e: e.dma_start(out=x1s.t[:], in_=x1_s[rows, :]), writes=[x1s])
                for k in range(8):
                    gk = gkr.next()
                    for hf in range(2):
                        dma("pool", lambda e: e.indirect_dma_start(
                            out=gk.t[:, hf * 512:(hf + 1) * 512], out_offset=None, in_=ys_s[hf],
                            in_offset=bass.IndirectOffsetOnAxis(ap=idx8.t[:, t, k:k + 1], axis=0),
                            bounds_check=bnd_rows, oob_is_err=False), reads=[idx8, ys_dep], writes=[gk])
                    if k == 0:
                        op("dve", lambda e: e.tensor_scalar(out=acc.t[:], in0=gk.t[:], scalar1=w8.t[:, t, 0:1], scalar2=None, op0=ALU.mult),
                           reads=[gk, w8], writes=[acc])
                    else:
                        op("dve", lambda e: e.scalar_tensor_tensor(out=acc.t[:], in0=gk.t[:], scalar=w8.t[:, t, k:k + 1], in1=acc.t[:],
                                                                   op0=ALU.mult, op1=ALU.add), reads=[gk, w8, acc], writes=[acc])
                op("dve", lambda e: e.tensor_tensor(out=acc.t[:], in0=acc.t[:], in1=gate2.t[:], op=ALU.mult), reads=[acc, gate2], writes=[acc])
                op("pool", lambda e: e.tensor_tensor(out=acc.t[:], in0=acc.t[:], in1=x1s.t[:], op=ALU.add), reads=[acc, x1s], writes=[acc])
                dma("sp", lambda e: e.dma_start(out=out_d[rows, :], in_=acc.t[:]), reads=[acc], writes=[out_dep])
            sc.barrier()
        sc.finalize("sp")
    return nc


def _rope_tables(S):
    t = np.arange(S)
    row = (t // 64).astype(np.float32)
    col = (t % 64).astype(np.float32)
    freqs = (10000.0 ** (-np.arange(16, dtype=np.float32) / 16)).astype(np.float32)
    ar = row[:, None] * freqs[None, :]
    ac = col[:, None] * freqs[None, :]
    cr, sr, cc, sn = np.cos(ar), np.sin(ar), np.cos(ac), np.sin(ac)
    tab = np.zeros((S + CTX, 2, 64), np.float32)
    tab[:S, 0] = np.concatenate([cr, cr, cc, cc], 1)
    tab[:S, 1] = np.concatenate([-sr, sr, -sn, sn], 1)
    tab[S:, 0] = 1.0
    return tab


def _consts():
    k = np.arange(128)[:, None]
    q = np.arange(128)[None, :]
    c = np.zeros((128, 5, 128), np.float32)
    c[:, 0] = (k == q)
    c[:, 1] = (k >= q)
    c[:, 2] = (k <= q)
    c[:, 3] = (k < q)
    c[:, 4] = 1.0
    return c


def prep_core(inp, b, S):
    f = lambda a: np.ascontiguousarray(a, dtype=np.float32)
    rep = lambda v: f(np.broadcast_to(np.asarray(v).reshape(1, -1), (128, np.asarray(v).size)))
    NBLK = (S * 8) // 128 + NE
    NB128 = (NBLK + 127) // 128
    d = {}
    d["x"] = f(inp["x"][b])
    d["ctx"] = f(inp["ctx"][b])
    d["cvec"] = f(np.stack([inp["c"][b].reshape(8, 128).T, inp["c_ctx"].reshape(8, 128).T], -1))
    d["w_ada"] = f(inp["w_ada"][0])
    d["b_ada"] = f(inp["b_ada"][0].reshape(48, 128).T)
    d["n12"] = f(np.stack([inp["norm1_g"][0].reshape(8, 128).T, inp["norm2_g"][0].reshape(8, 128).T], 1))
    d["w_in"] = f(inp["w_in"][0])
    d["gain"] = rep(np.concatenate([np.tile(inp["knorm_a"][0], 2), np.tile(inp["knorm_b"][0], 16),
                                    np.tile(inp["qnorm_a"][0], 16), np.tile(inp["qnorm_b"][0], 16)]))
    d["rope"] = _rope_tables(S)
    d["sink"] = rep(inp["sink_a"][0].reshape(-1))
    d["lamv"] = f(rep(np.stack([inp["lam_q1"][0], inp["lam_k1"][0], inp["lam_q2"][0], inp["lam_k2"][0]]).reshape(-1)).reshape(128, 4, 64))
    d["subg"] = rep(inp["subln_g"][0])
    d["w_pa"] = f(inp["w_pa"][0])
    d["w_pb"] = f(inp["w_pb"][0])
    d["w_o"] = f(inp["w_o"][0])
    d["w_router"] = f(inp["w_router"][0])
    d["rbias"] = rep(inp["router_bias"][0])
    d["w_gate_e"] = f(inp["w_gate_e"][0]).reshape(NE * 128, 2048)
    d["w_up_e"] = f(inp["w_up_e"][0]).reshape(NE * 128, 2048)
    d["w_down_e"] = f(inp["w_down_e"][0]).reshape(NE * 128, 2048)
    d["w_gate_s"] = f(inp["w_gate_s"][0])
    d["w_up_s"] = f(inp["w_up_s"][0])
    d["w_down_s"] = f(inp["w_down_s"][0])
    d["consts"] = _consts()
    d["thr"] = f((128.0 * (np.arange(NB128)[None, :] * 128 + np.arange(128)[:, None])))
    return d


def kernel(**inputs):
    inp = {k: np.asarray(v) for k, v in inputs.items()}
    B, S, _ = inp["x"].shape
    nc = build(S)
    in_maps = [prep_core(inp, b, S) for b in range(B)]
    res = run_bass_kernel_spmd(nc, in_maps, core_ids=list(range(B)))
    return np.stack([np.asarray(r["out"], dtype=np.float32) for r in res.results], 0)
```

```python
import contextlib
import math
import numpy as np
import concourse.bass as bass
import concourse.mybir as mybir
from concourse.bass_utils import run_bass_kernel_spmd

F32 = mybir.dt.float32
BF16 = mybir.dt.bfloat16
I32 = mybir.dt.int32
ALU = mybir.AluOpType
AF = mybir.ActivationFunctionType
AX = mybir.AxisListType

D = 1024
CTX = 256
NE = 256
EPS = 1e-6
LAM_INIT = 0.8 - 0.6 * math.exp(-0.3 * 0)


class Dep:
    __slots__ = ("w", "r")

    def __init__(self):
        self.w = None
        self.r = {}


class Buf:
    __slots__ = ("t", "d", "psum")

    def __init__(self, t, psum=False):
        self.t = t
        self.d = Dep()
        self.psum = psum


class Sched:
    def __init__(self, nc, es, ndma=20):
        self.nc = nc
        self.eng = {"pe": nc.tensor, "act": nc.scalar, "dve": nc.vector,
                    "pool": nc.gpsimd, "sp": nc.sync}
        self.sem = {}
        self.cnt = {}
        self.pending = {}
        self.seen = {k: {} for k in self.eng}
        for k in self.eng:
            self.sem[k] = es.enter_context(nc.semaphore("s_" + k))
            self.cnt[k] = 0
            self.pending[k] = False
        self.dq = {}
        for q in ("sp", "act", "pool"):
            sl = []
            for i in range(ndma):
                key = "d_%s_%d" % (q, i)
                self.sem[key] = es.enter_context(nc.semaphore(key))
                self.cnt[key] = 0
                sl.append(key)
            self.dq[q] = [sl, 0]
        self.ninst = 0

    def _wait(self, e, key, val):
        if val <= 0 or (e == "pe" and key == "pe"):
            return
        if self.seen[e].get(key, 0) >= val:
            return
        self.eng[e].wait_ge(self.sem[key], val)
        self.seen[e][key] = val

    def _deps(self, e, reads, writes):
        need = {}
        for b in reads:
            if b.w is not None:
                k, v = b.w
                if need.get(k, 0) < v:
                    need[k] = v
        for b in writes:
            if b.w is not None:
                k, v = b.w
                if need.get(k, 0) < v:
                    need[k] = v
            for k, v in b.r.items():
                if need.get(k, 0) < v:
                    need[k] = v
        for k, v in need.items():
            if k == e and v > self.cnt[e]:
                continue
            self._wait(e, k, v)

    def op(self, e, fn, reads=(), writes=(), inc=True):
        writes = list(writes) + [b for b in reads if isinstance(b, Buf) and b.psum and b not in writes]
        reads = [b for b in reads if not (isinstance(b, Buf) and b.psum)]
        reads = [b.d if isinstance(b, Buf) else b for b in reads]
        writes = [b.d if isinstance(b, Buf) else b for b in writes]
        self._deps(e, reads, writes)
        inst = fn(self.eng[e])
        self.ninst += 1
        val = self.cnt[e] + 1
        if inc:
            inst.then_inc(self.sem[e], 1)
            self.cnt[e] = val
            self.pending[e] = False
        else:
            self.pending[e] = True
        for b in reads:
            if b.r.get(e, 0) < val:
                b.r[e] = val
        for b in writes:
            b.w = (e, val)
            b.r = {}
        return inst

    def dma(self, q, fn, reads=(), writes=()):
        reads = [b.d if isinstance(b, Buf) else b for b in reads]
        writes = [b.d if isinstance(b, Buf) else b for b in writes]
        assert not self.pending[q]
        sl, i = self.dq[q]
        key = sl[i % len(sl)]
        self.dq[q][1] = i + 1
        self._wait(q, key, self.cnt[key])
        self._deps(q, reads, writes)
        inst = fn(self.eng[q])
        self.ninst += 1
        self.cnt[key] += 16
        val = self.cnt[key]
        inst.then_inc(self.sem[key], 16)
        for b in reads:
            if b.r.get(key, 0) < val:
                b.r[key] = val
        for b in writes:
            b.w = (key, val)
            b.r = {}
        return inst

    def barrier(self):
        for e in self.eng:
            assert not self.pending[e], e
        for e in self.eng:
            for key, v in self.cnt.items():
                if key != e:
                    self._wait(e, key, v)

    def finalize(self, e="sp"):
        for key, v in self.cnt.items():
            if key != e and v > 0:
                self._wait(e, key, v)


class Ring:
    def __init__(self, bufs):
        self.bufs = bufs
        self.i = 0

    def next(self):
        b = self.bufs[self.i % len(self.bufs)]
        self.i += 1
        return b


def build(S, debug=False, stopat=None):
    NT = S // 128
    NKT = NT + 2
    SK = S + CTX
    QC = min(512, S)
    NQC = S // QC
    NQS = QC // 128
    NBLK = (S * 8) // 128 + NE
    NB128 = (NBLK + 127) // 128
    assert NBLK % 16 == 0

    nc = bass.Bass("TRN2", target_bir_lowering=False)

    def din(name, shape, dt=F32):
        return nc.dram_tensor(name, list(shape), dt, kind="ExternalInput").ap()

    dbg_kind = "ExternalOutput"

    def dscr(name, shape, dt):
        return nc.dram_tensor(name, list(shape), dt, kind=dbg_kind).ap()

    x_d = din("x", [S, D])
    ctx_d = din("ctx", [CTX, D])
    cvec_d = din("cvec", [128, 8, 2])
    wada_d = din("w_ada", [D, 6 * D])
    bada_d = din("b_ada", [128, 48])
    n12_d = din("n12", [128, 2, 8])
    win_d = din("w_in", [D, 6400])
    gain_d = din("gain", [128, 3200])
    rope_d = din("rope", [SK, 2, 64])
    sink_d = din("sink", [128, 16])
    lamv_d = din("lamv", [128, 4, 64])
    subg_d = din("subg", [128, 128])
    wpa_d = din("w_pa", [D, D])
    wpb_d = din("w_pb", [D, D])
    wo_d = din("w_o", [D, D])
    wr_d = din("w_router", [D, NE])
    rb_d = din("rbias", [128, NE])
    _er = NE * 128 if stopat is None else 128
    wge_d = din("w_gate_e", [_er, 2048])
    wue_d = din("w_up_e", [_er, 2048])
    wde_d = din("w_down_e", [_er, 2048])
    wgs_d = din("w_gate_s", [D, 256])
    wus_d = din("w_up_s", [D, 256])
    wds_d = din("w_down_s", [256, D])
    cst_d = din("consts", [128, 5, 128])
    thr_d = din("thr", [128, NB128])
    out_d = nc.dram_tensor("out", [S, D], F32, kind="ExternalOutput").ap()

    mod_s = dscr("mod_s", [8, D], F32)
    kaT_s = dscr("kaT_s", [128, SK], BF16)
    va_s = dscr("va_s", [SK, 128], BF16)
    kbT_s = dscr("kbT_s", [D, SK], BF16)
    vb_s = dscr("vb_s", [SK, D], BF16)
    qaT_s = dscr("qaT_s", [D, S], BF16)
    qbT_s = dscr("qbT_s", [D, S], BF16)
    g_s = dscr("g_s", [S, 2 * D], BF16)
    oaT_s = dscr("oaT_s", [D, S], BF16)
    obT_s = dscr("obT_s", [D, S], BF16)
    x1_s = dscr("x1_s", [S, D], F32)
    h2_s = dscr("h2_s", [S, D], BF16)
    rank_s = dscr("rank_s", [S, NE], F32)
    wt_s = dscr("wt_s", [S, NE], F32)
    eb_s = dscr("eb_s", [1, NB128 * 128], F32)
    xg_s = dscr("xg_s", [NBLK * 128, D], BF16)
    ys_s = [dscr("ys%d_s" % i, [NBLK * 128, 512], F32) for i in range(2)]

    with contextlib.ExitStack() as es0:
        sc = Sched(nc, es0)
        op, dma = sc.op, sc.dma

        def mk(es, name, shape, dt):
            return Buf(es.enter_context(nc.sbuf_tensor("sb_" + name, list(shape), dt)))

        def mkring(es, name, shape, dt, n):
            return Ring([mk(es, "%s%d" % (name, i), shape, dt) for i in range(n)])

        def mkps(es, name, shape, dt):
            return Buf(es.enter_context(nc.psum_tensor("ps_" + name, list(shape), dt)), psum=True)

        def mkpsring(es, name, shape, dt, n):
            return Ring([mkps(es, "%s%d" % (name, i), shape, dt) for i in range(n)])

        cst = mk(es0, "cst", [128, 5, 128], BF16)
        dma("pool", lambda e: e.dma_start(out=cst.t[:], in_=cst_d), writes=[cst])
        ident = cst.t[:, 0, :]
        m_prev = cst.t[:, 1, :]
        m_next = cst.t[:, 2, :]
        tri = cst.t[:, 3, :]
        ones_bf = cst.t[:, 4, :]
        epsb = mk(es0, "epsb", [128, 1], F32)
        op("pool", lambda e: e.memset(epsb.t[:], EPS), writes=[epsb])
        idx8 = mk(es0, "idx8", [128, NT, 8], I32)
        w8 = mk(es0, "w8", [128, NT, 8], F32)
        idxw = mk(es0, "idxw", [128, NBLK], I32)
        _r1 = es0.enter_context(nc.gpsimd.register("bnd_rows"))
        nc.gpsimd.reg_mov(_r1, NBLK * 128 - 1)
        bnd_rows = nc.gpsimd.snap(_r1)
        _r2 = es0.enter_context(nc.gpsimd.register("bnd_w"))
        nc.gpsimd.reg_mov(_r2, NE * 128 - 1)
        bnd_w = nc.gpsimd.snap(_r2)

        def rsqrt_mean(dst, src, n, rd, wr):
            op("act", lambda e: e.activation(out=dst, in_=src, func=AF.Ln, scale=1.0 / n, bias=epsb.t[:, 0:1]),
               reads=rd + [epsb], writes=wr)
            op("act", lambda e: e.activation(out=dst, in_=dst, func=AF.Exp, scale=-0.5), reads=wr, writes=wr)

        with contextlib.ExitStack() as es:
            cv = mk(es, "cv", [128, 8, 2], F32)
            cs = mk(es, "cs", [128, 8, 2], F32)
            bada = mk(es, "bada", [128, 48], F32)
            n12 = mk(es, "n12", [128, 2, 8], F32)
            modv = mk(es, "modv", [128, 48, 2], F32)
            mv8 = mk(es, "mv8", [128, 8, 8], F32)
            war = mkring(es, "wa", [128, 8, 1024], F32, 2)
            pmod = mkps(es, "pmod", [128, 48, 2], F32)
            dma("sp", lambda e: e.dma_start(out=cv.t[:], in_=cvec_d), writes=[cv])
            dma("sp", lambda e: e.dma_start(out=bada.t[:], in_=bada_d), writes=[bada])
            dma("sp", lambda e: e.dma_start(out=n12.t[:], in_=n12_d), writes=[n12])
            op("act", lambda e: e.activation(out=cs.t[:], in_=cv.t[:], func=AF.Silu), reads=[cv], writes=[cs])
            for m6 in range(6):
                wa = war.next()
                dma("sp" if m6 % 2 == 0 else "act", lambda e: e.dma_start(
                    out=wa.t[:], in_=wada_d[:, m6 * 1024:(m6 + 1) * 1024].rearrange("(j p) n -> p j n", p=128)),
                    writes=[wa])
                for mm in range(8):
                    for j in range(8):
                        op("pe", lambda e: e.matmul(pmod.t[:, m6 * 8 + mm, :], lhsT=wa.t[:, j, mm * 128:(mm + 1) * 128],
                                                    rhs=cs.t[:, j, :], start=(j == 0), stop=(j == 7)),
                           reads=[wa, cs], writes=[pmod], inc=(j == 7))
            op("dve", lambda e: e.tensor_tensor(out=modv.t[:], in0=pmod.t[:],
                                                in1=bada.t[:].unsqueeze(2).to_broadcast([128, 48, 2]), op=ALU.add),
               reads=[pmod, bada], writes=[modv])
            def mvs(i):
                return mv8.t[:, :, i]
            def modc(ch, who):
                return modv.t[:, ch * 8:(ch + 1) * 8, who]
            def affine_gain(dst, scl, ng):
                op("dve", lambda e: e.scalar_tensor_tensor(out=dst, in0=scl, scalar=1.0, in1=ng, op0=ALU.add, op1=ALU.mult),
                   reads=[modv, n12], writes=[mv8])
            affine_gain(mvs(0), modc(1, 0), n12.t[:, 0, :])
            op("dve", lambda e: e.tensor_copy(out=mvs(1), in_=modc(0, 0)), reads=[modv], writes=[mv8])
            affine_gain(mvs(2), modc(1, 1), n12.t[:, 0, :])
            op("dve", lambda e: e.tensor_copy(out=mvs(3), in_=modc(0, 1)), reads=[modv], writes=[mv8])
            affine_gain(mvs(4), modc(4, 0), n12.t[:, 1, :])
            op("dve", lambda e: e.tensor_copy(out=mvs(5), in_=modc(3, 0)), reads=[modv], writes=[mv8])
            op("dve", lambda e: e.tensor_copy(out=mvs(6), in_=modc(2, 0)), reads=[modv], writes=[mv8])
            op("dve", lambda e: e.tensor_copy(out=mvs(7), in_=modc(5, 0)), reads=[modv], writes=[mv8])
            mod_dep = Dep()
            for i in range(8):
                dma("sp", lambda e: e.dma_start(out=mod_s[i].rearrange("(j p) -> p j", p=128), in_=mv8.t[:, :, i],
                                                allow_slow_non_contiguous=True),
                    reads=[mv8], writes=[mod_dep])
            sc.barrier()
        if stopat == 'A':
            sc.finalize("sp")
            return nc

        def load_bc(es, name, i, q="sp"):
            b = mk(es, name, [128, D], F32)
            dma(q, lambda e: e.dma_start(out=b.t[:], in_=mod_s[i:i + 1, :].partition_broadcast(128)),
                reads=[mod_dep], writes=[b])
            return b

        def norm_affine(xt, gbc, shbc, ssr, junkr, tmpr, hr):
            ss = ssr.next()
            junk = junkr.next()
            tmp = tmpr.next()
            h = hr.next()
            op("pool", lambda e: e.memset(ss.t[:], 0.0), writes=[ss])
            op("act", lambda e: e.activation(out=junk.t[:], in_=xt.t[:], func=AF.Square, accum_out=ss.t[:, 0:1]),
               reads=[xt, ss], writes=[junk, ss])
            rsqrt_mean(ss.t[:, 0:1], ss.t[:, 0:1], D, [ss], [ss])
            op("dve", lambda e: e.scalar_tensor_tensor(out=tmp.t[:], in0=xt.t[:], scalar=ss.t[:, 0:1], in1=gbc.t[:],
                                                       op0=ALU.mult, op1=ALU.mult), reads=[xt, ss, gbc], writes=[tmp])
            op("pool", lambda e: e.tensor_tensor(out=h.t[:], in0=tmp.t[:], in1=shbc.t[:], op=ALU.add),
               reads=[tmp, shbc], writes=[h])
            return h

        def transpose8(src, ptr, dstr, evac="act"):
            pt = ptr.next()
            dst = dstr.next()
            for j in range(8):
                op("pe", lambda e: e.transpose(out=pt.t[:, j, :], in_=src.t[:, j * 128:(j + 1) * 128], identity=ident),
                   reads=[src, cst], writes=[pt], inc=(j == 7))
            if evac == "act":
                op("act", lambda e: e.copy(out=dst.t[:], in_=pt.t[:]), reads=[pt], writes=[dst])
            else:
                op("dve", lambda e: e.tensor_copy(out=dst.t[:], in_=pt.t[:]), reads=[pt], writes=[dst])
            return dst

        with contextlib.ExitStack() as es:
            g1bc = load_bc(es, "g1bc", 0)
            sh1bc = load_bc(es, "sh1bc", 1, "act")
            g1cbc = load_bc(es, "g1cbc", 2)
            sh1cbc = load_bc(es, "sh1cbc", 3, "act")
            win = mk(es, "win", [128, 8, 6400], BF16)
            for j in range(8):
                for c4 in range(4):
                    dma("pool", lambda e: e.dma_start(out=win.t[:, j, c4 * 1600:(c4 + 1) * 1600],
                                                      in_=win_d[j * 128:(j + 1) * 128, c4 * 1600:(c4 + 1) * 1600]),
                        writes=[win])
            gain = mk(es, "gain", [128, 3200], F32)
            dma("sp", lambda e: e.dma_start(out=gain.t[:], in_=gain_d), writes=[gain])
            xr = mkring(es, "xt", [128, D], F32, 2)
            ssr = mkring(es, "ss", [128, 1], F32, 2)
            junkr = mkring(es, "junk", [128, D], BF16, 1)
            tmpr = mkring(es, "tmp", [128, D], F32, 1)
            hr = mkring(es, "h", [128, D], BF16, 2)
            hTr = mkring(es, "hT", [128, 8, 128], BF16, 2)
            roper = mkring(es, "rope", [128, 2, 64], F32, 2)
            sqr = mkring(es, "sq", [128, 512], F32, 2)
            ssgr = mkring(es, "ssg", [128, 8], F32, 2)
            y32r = mkring(es, "y32", [128, 512], F32, 2)
            t1r = mkring(es, "t1", [128, 512], F32, 2)
            t2r = mkring(es, "t2", [128, 512], F32, 2)
            qkbr = mkring(es, "qkb", [128, 512], BF16, 2)
            stgr = mkring(es, "stg", [128, 4, 128], BF16, 3)
            vstr = mkring(es, "vst", [128, 512], BF16, 3)
            ptr8 = mkpsring(es, "ptr8", [128, 8, 128], BF16, 1)
            ptr4 = mkpsring(es, "ptr4", [128, 4, 128], BF16, 1)
            ppr = mkpsring(es, "pp", [128, 512], F32, 5)
            scr_dep = {k: Dep() for k in ("kaT", "kbT", "vb", "qaT", "qbT", "g")}
            scr_dep_va = Dep()

            segs = [(0, 256, "kava", 0, None, 0),
                    (256, 512, "qk", 128, "kbT", 0), (768, 512, "qk", 640, "kbT", 512),
                    (1280, 512, "v", 0, "vb", 0), (1792, 512, "v", 0, "vb", 512),
                    (2304, 512, "qk", 1152, "qaT", 0), (2816, 512, "qk", 1664, "qaT", 512),
                    (3328, 512, "qk", 2176, "qbT", 0), (3840, 512, "qk", 2688, "qbT", 512),
                    (4352, 512, "g", 0, "g", 0), (4864, 512, "g", 0, "g", 512),
                    (5376, 512, "g", 0, "g", 1024), (5888, 512, "g", 0, "g", 1536)]
            scr_ap = {"kbT": kbT_s, "qaT": qaT_s, "qbT": qbT_s}
            dq = ["sp", "act"]
            dqi = [0]

            def nextq():
                dqi[0] += 1
                return dq[dqi[0] % 2]

            def qk_post(pp, w, gcol, rope, t, which, row0):
                G = w // 64
                sq = sqr.next(); ssg = ssgr.next(); y32 = y32r.next(); t1 = t1r.next(); t2 = t2r.next(); qkb = qkbr.next()
                op("act", lambda e: e.activation(out=sq.t[:, 0:w], in_=pp.t[:, 0:w], func=AF.Square), reads=[pp], writes=[sq])
                op("dve", lambda e: e.tensor_tensor(out=y32.t[:, 0:w], in0=pp.t[:, 0:w], in1=gain.t[:, gcol:gcol + w], op=ALU.mult),
                   reads=[pp, gain], writes=[y32])
                op("dve", lambda e: e.tensor_reduce(out=ssg.t[:, 0:G], in_=sq.t[:, 0:w].rearrange("p (g d) -> p g d", d=64),
                                                    axis=AX.X, op=ALU.add), reads=[sq], writes=[ssg])
                rsqrt_mean(ssg.t[:, 0:G], ssg.t[:, 0:G], 64, [ssg], [ssg])
                if stopat == "Q1":
                    return
                yv = y32.t[:, 0:w].rearrange("p (g d) -> p g d", d=64)
                op("dve", lambda e: e.tensor_tensor(out=t1.t[:, 0:w].rearrange("p (g d) -> p g d", d=64), in0=yv,
                                                    in1=rope.t[:, 0, :].unsqueeze(1).to_broadcast([128, G, 64]), op=ALU.mult),
                   reads=[y32, rope], writes=[t1])
                if stopat == "Q2":
                    return
                y5 = y32.t[:, 0:w].rearrange("p (g a b f) -> p (g a) b f", a=2, b=2, f=16)
                t5 = t2.t[:, 0:w].rearrange("p (g a b f) -> p (g a) b f", a=2, b=2, f=16)
                s5 = rope.t[:, 1, :].rearrange("p (a b f) -> p a b f", a=2, b=2, f=16)
                for a in range(2):
                    ya = y32.t[:, 0:w].rearrange("p (g a b f) -> p g a b f", a=2, b=2, f=16)[:, :, a]
                    ta = t2.t[:, 0:w].rearrange("p (g a b f) -> p g a b f", a=2, b=2, f=16)[:, :, a]
                    for b in range(2):
                        op("pool", lambda e: e.tensor_tensor(out=ta[:, :, b, :], in0=ya[:, :, 1 - b, :],
                                                             in1=s5[:, a, b, :].unsqueeze(1).to_broadcast([128, G, 16]), op=ALU.mult),
                           reads=[y32, rope], writes=[t2])
                op("pool", lambda e: e.tensor_tensor(out=t1.t[:, 0:w], in0=t1.t[:, 0:w], in1=t2.t[:, 0:w], op=ALU.add),
                   reads=[t1, t2], writes=[t1])
                if stopat == "Q3":
                    return
                op("dve", lambda e: e.tensor_tensor(out=qkb.t[:, 0:w].rearrange("p (g d) -> p g d", d=64),
                                                    in0=t1.t[:, 0:w].rearrange("p (g d) -> p g d", d=64),
                                                    in1=ssg.t[:, 0:G].unsqueeze(2).to_broadcast([128, G, 64]), op=ALU.mult),
                   reads=[t1, ssg], writes=[qkb])
                if stopat == "Q4":
                    return
                nb = w // 128
                pt = ptr4.next(); stg = stgr.next()
                for j in range(nb):
                    op("pe", lambda e: e.transpose(out=pt.t[:, j, :], in_=qkb.t[:, j * 128:(j + 1) * 128], identity=ident),
                       reads=[qkb, cst], writes=[pt], inc=(j == nb - 1))
                op("act", lambda e: e.copy(out=stg.t[:, 0:nb, :], in_=pt.t[:, 0:nb, :]), reads=[pt], writes=[stg])
                if which == "kaT":
                    dma(nextq(), lambda e: e.dma_start(out=kaT_s[:, t * 128:(t + 1) * 128], in_=stg.t[:, 0, :]),
                        reads=[stg], writes=[scr_dep["kaT"]])
                else:
                    dst = scr_ap[which][row0:row0 + w, t * 128:(t + 1) * 128].rearrange("(j p) n -> p j n", p=128)
                    dma(nextq(), lambda e: e.dma_start(out=dst, in_=stg.t[:, 0:nb, :]), reads=[stg], writes=[scr_dep[which]])

            if stopat == "B0":
                sc.barrier(); sc.finalize("sp"); return nc
            for t in range(NKT):
                is_ctx = t >= NT
                xt = xr.next()
                src = ctx_d[(t - NT) * 128:(t - NT + 1) * 128, :] if is_ctx else x_d[t * 128:(t + 1) * 128, :]
                dma("sp", lambda e: e.dma_start(out=xt.t[:], in_=src), writes=[xt])
                rope = roper.next()
                dma("act", lambda e: e.dma_start(out=rope.t[:], in_=rope_d[t * 128:(t + 1) * 128]), writes=[rope])
                h = norm_affine(xt, g1cbc if is_ctx else g1bc, sh1cbc if is_ctx else sh1bc, ssr, junkr, tmpr, hr)
                hT = transpose8(h, ptr8, hTr)
                for (c0, w, kind, gcol, which, row0) in segs:
                    if is_ctx and c0 >= 2304:
                        continue
                    if stopat == "B1" or (stopat == "B2" and kind in ("kava", "qk")):
                        continue
                    pp = ppr.next()
                    for j in range(8):
                        op("pe", lambda e: e.matmul(pp.t[:, 0:w], lhsT=hT.t[:, j, :], rhs=win.t[:, j, c0:c0 + w],
                                                    start=(j == 0), stop=(j == 7)), reads=[hT, win], writes=[pp], inc=(j == 7))
                    if kind == "kava":
                        qk_post(pp, 128, 0, rope, t, "kaT", 0)
                        vst = vstr.next()
                        op("act", lambda e: e.copy(out=vst.t[:, 0:128], in_=pp.t[:, 128:256]), reads=[pp], writes=[vst])
                        dma(nextq(), lambda e: e.dma_start(out=va_s[t * 128:(t + 1) * 128, :], in_=vst.t[:, 0:128]),
                            reads=[vst], writes=[scr_dep_va])
                    elif kind == "qk":
                        qk_post(pp, w, gcol, rope, t, which, row0)
                    elif kind == "v":
                        vst = vstr.next()
                        op("act", lambda e: e.copy(out=vst.t[:], in_=pp.t[:]), reads=[pp], writes=[vst])
                        dma(nextq(), lambda e: e.dma_start(out=vb_s[t * 128:(t + 1) * 128, row0:row0 + 512], in_=vst.t[:]),
                            reads=[vst], writes=[scr_dep["vb"]])
                    else:
                        vst = vstr.next()
                        op("act", lambda e: e.activation(out=vst.t[:], in_=pp.t[:], func=AF.Sigmoid), reads=[pp], writes=[vst])
                        dma(nextq(), lambda e: e.dma_start(out=g_s[t * 128:(t + 1) * 128, row0:row0 + 512], in_=vst.t[:]),
                            reads=[vst], writes=[scr_dep["g"]])
            sc.barrier()
        if stopat in ("B", "B1", "B2", "Q1", "Q2", "Q3", "Q4"):
            sc.finalize("sp")
            return nc

        with contextlib.ExitStack() as es:
            kaT = mk(es, "kaT", [64, 2, SK], BF16)
            dma("sp", lambda e: e.dma_start(out=kaT.t[:], in_=kaT_s.rearrange("(g d) n -> d g n", d=64)), writes=[kaT])
            va = mk(es, "va", [128, NKT, 2, 65], BF16)
            op("pool", lambda e: e.memset(va.t[:], 1.0), writes=[va])
            for g_ in range(2):
                for t0 in range(0, NKT, 8):
                    t1_ = min(NKT, t0 + 8)
                    dma("act" if g_ == 0 else "sp", lambda e: e.dma_start(
                        out=va.t[:, t0:t1_, g_, 0:64],
                        in_=va_s[t0 * 128:t1_ * 128, g_ * 64:(g_ + 1) * 64].rearrange("(t p) e -> p t e", p=128)), writes=[va])
            esink = mk(es, "esink", [128, 16], F32)
            dma("sp", lambda e: e.dma_start(out=esink.t[:], in_=sink_d), writes=[esink])
            op("act", lambda e: e.activation(out=esink.t[:], in_=esink.t[:], func=AF.Exp), reads=[esink], writes=[esink])
            qar = mkring(es, "qa", [64, 8, 128], BF16, 2)
            pTr = mkring(es, "pTa", [128, 1024], BF16, 2)
            psr = mkpsring(es, "psa", [128, 1024], F32, 2)
            po = [mkps(es, "poa%d" % i, [128, 512], F32) for i in range(2)]
            ptr8 = mkpsring(es, "ptr8d", [128, 8, 128], BF16, 1)
            oatr = mkring(es, "oatok", [128, D], BF16, 2)
            zr = mkring(es, "zz", [128, 16], F32, 2)
            oaTr = mkring(es, "oaT", [128, 8, 128], BF16, 2)
            oaT_dep = Dep()
            for i in range(NT):
                oat = oatr.next(); zz = zr.next()
                for g in range(2):
                    qa = qar.next()
                    dma("sp" if g == 0 else "act", lambda e: e.dma_start(
                        out=qa.t[:], in_=qaT_s[g * 512:(g + 1) * 512, i * 128:(i + 1) * 128].rearrange("(h d) n -> d h n", d=64)),
                        writes=[qa])
                    kts = [(t, t - i) for t in (i - 1, i, i + 1) if 0 <= t < NT] + [(NT, 0), (NT + 1, 0)]
                    for ki, (kt, rel) in enumerate(kts):
                        ps = psr.next(); pT = pTr.next()
                        for hf in range(2):
                            op("pe", lambda e: e.matmul(ps.t[:, hf * 512:(hf + 1) * 512], lhsT=kaT.t[:, g, kt * 128:(kt + 1) * 128],
                                                        rhs=qa.t[:, hf * 4:(hf + 1) * 4, :], start=True, stop=True),
                               reads=[kaT, qa], writes=[ps], inc=(hf == 1))
                        op("act", lambda e: e.activation(out=pT.t[:], in_=ps.t[:], func=AF.Exp, scale=0.125), reads=[ps], writes=[pT])
                        if rel != 0:
                            msk = m_prev if rel < 0 else m_next
                            pv3 = pT.t[:].rearrange("p (h q) -> p h q", q=128)
                            op("pool", lambda e: e.tensor_tensor(out=pv3, in0=pv3, in1=msk.unsqueeze(1).to_broadcast([128, 8, 128]),
                                                                 op=ALU.mult), reads=[pT, cst], writes=[pT])
                        for hh in range(8):
                            pob = po[hh // 4]
                            c0 = (hh % 4) * 65
                            op("pe", lambda e: e.matmul(pob.t[:, c0:c0 + 65], lhsT=pT.t[:, hh * 128:(hh + 1) * 128], rhs=va.t[:, kt, g, :],
                                                        start=(ki == 0 and hh % 4 == 0), stop=(ki == len(kts) - 1), skip_group_check=True),
                               reads=[pT, va], writes=[pob], inc=(hh % 4 == 3))
                    for half in range(2):
                        pv = po[half].t[:, 0:260].rearrange("p (h e) -> p h e", e=65)
                        h0 = g * 8 + half * 4
                        op("dve", lambda e: e.tensor_tensor(out=zz.t[:, h0:h0 + 4], in0=pv[:, :, 64], in1=esink.t[:, h0:h0 + 4], op=ALU.add),
                           reads=[po[half], esink], writes=[zz])
                        op("dve", lambda e: e.reciprocal(out=zz.t[:, h0:h0 + 4], in_=zz.t[:, h0:h0 + 4]), reads=[zz], writes=[zz])
                        op("dve", lambda e: e.tensor_tensor(out=oat.t[:, h0 * 64:(h0 + 4) * 64].rearrange("p (h e) -> p h e", e=64),
                                                            in0=pv[:, :, 0:64], in1=zz.t[:, h0:h0 + 4].unsqueeze(2).to_broadcast([128, 4, 64]),
                                                            op=ALU.mult), reads=[po[half], zz], writes=[oat])
                oaT = transpose8(oat, ptr8, oaTr)
                dma("sp", lambda e: e.dma_start(out=oaT_s[:, i * 128:(i + 1) * 128].rearrange("(j p) n -> p j n", p=128), in_=oaT.t[:]),
                    reads=[oaT], writes=[oaT_dep])
            sc.barrier()
        if stopat == 'D':
            sc.finalize("sp")
            return nc

        with contextlib.ExitStack() as es:
            zt = mk(es, "zt", [128, 4096], BF16)
            op("pool", lambda e: e.memset(zt.t[:], 0.0), writes=[zt])
            xg_dep = Dep()
            xgz = xg_s.rearrange("(a p r) n -> a p (r n)", p=128, r=4)
            for a in range(NBLK // 4):
                dma("pool", lambda e: e.dma_start(out=xgz[a], in_=zt.t[:]), reads=[zt], writes=[xg_dep])
            lamv = mk(es, "lamv", [128, 4, 64], F32)
            lam = mk(es, "lam", [128, 4], F32)
            ljunk = mk(es, "ljunk", [128, 64], F32)
            dma("sp", lambda e: e.dma_start(out=lamv.t[:], in_=lamv_d), writes=[lamv])
            op("pool", lambda e: e.memset(lam.t[:], 0.0), writes=[lam])
            for i2 in range(2):
                op("dve", lambda e: e.tensor_tensor(out=ljunk.t[:], in0=lamv.t[:, 2 * i2, :], in1=lamv.t[:, 2 * i2 + 1, :], op=ALU.mult),
                   reads=[lamv], writes=[ljunk])
                op("dve", lambda e: e.tensor_reduce(out=lam.t[:, i2:i2 + 1], in_=ljunk.t[:], axis=AX.X, op=ALU.add),
                   reads=[ljunk], writes=[lam])
            op("act", lambda e: e.activation(out=lam.t[:, 0:2], in_=lam.t[:, 0:2], func=AF.Exp), reads=[lam], writes=[lam])
            op("dve", lambda e: e.tensor_tensor(out=lam.t[:, 2:3], in0=lam.t[:, 1:2], in1=lam.t[:, 0:1], op=ALU.subtract), reads=[lam], writes=[lam])
            op("dve", lambda e: e.tensor_scalar(out=lam.t[:, 2:3], in0=lam.t[:, 2:3], scalar1=-LAM_INIT, scalar2=None, op0=ALU.add),
               reads=[lam], writes=[lam])
            subg = mk(es, "subg", [128, 128], F32)
            dma("sp", lambda e: e.dma_start(out=subg.t[:], in_=subg_d), writes=[subg])
            op("dve", lambda e: e.tensor_scalar(out=subg.t[:], in0=subg.t[:], scalar1=1.0 - LAM_INIT, scalar2=None, op0=ALU.mult),
               reads=[subg], writes=[subg])
            kbr = mkring(es, "kb", [64, 2, SK], BF16, 2)
            vbr = mkring(es, "vb", [128, NKT, 129], BF16, 2)
            for b_ in vbr.bufs:
                op("pool", lambda e: e.memset(b_.t[:], 1.0), writes=[b_])
            qbr = mkring(es, "qb", [64, 2, QC], BF16, 2)
            pTr = mkring(es, "pTe", [128, 2, QC], BF16, 2)
            psr = mkpsring(es, "pse", [128, 2, 512], F32, 2)
            accb = [mkps(es, "acc%d" % i, [128, 512], F32) for i in range(3)]
            ptb = mkps(es, "ptb", [128, 8, 128], BF16)
            rzr = mkring(es, "rz", [128, 2], F32, 2)
            tbr = mkring(es, "tb", [128, 128], F32, 2)
            dbr = mkring(es, "db", [128, 128], F32, 2)
            ssr = mkring(es, "sse", [128, 1], F32, 2)
            sjr = mkring(es, "sje", [128, 128], BF16, 1)
            obr = mkring(es, "obb", [128, 128], BF16, 2)
            ostr = mkring(es, "obst", [128, QC], BF16, 2)
            obT_dep = Dep()

            def accv(n):
                return accb[n // 3], accb[n // 3].t[:, (n % 3) * 129:(n % 3) * 129 + 129]

            for h in range(8):
                kb = kbr.next(); vb = vbr.next()
                dma("sp", lambda e: e.dma_start(out=kb.t[:], in_=kbT_s[h * 128:(h + 1) * 128, :].rearrange("(j d) n -> d j n", d=64)),
                    writes=[kb])
                for t0 in range(0, NKT, 8):
                    t1_ = min(NKT, t0 + 8)
                    dma("act", lambda e: e.dma_start(
                        out=vb.t[:, t0:t1_, 0:128],
                        in_=vb_s[t0 * 128:t1_ * 128, h * 128:(h + 1) * 128].rearrange("(t p) e -> p t e", p=128)), writes=[vb])
                for qc in range(NQC):
                    qb = qbr.next()
                    dma("sp", lambda e: e.dma_start(
                        out=qb.t[:], in_=qbT_s[h * 128:(h + 1) * 128, qc * QC:(qc + 1) * QC].rearrange("(j d) n -> d j n", d=64)),
                        writes=[qb])
                    for kt in range(NKT):
                        ps = psr.next(); pT = pTr.next()
                        for j in range(2):
                            op("pe", lambda e: e.matmul(ps.t[:, j, 0:QC], lhsT=kb.t[:, j, kt * 128:(kt + 1) * 128], rhs=qb.t[:, j, :],
                                                        start=True, stop=True), reads=[kb, qb], writes=[ps], inc=(j == 1))
                        op("act", lambda e: e.activation(out=pT.t[:], in_=ps.t[:, :, 0:QC], func=AF.Exp, scale=0.125),
                           reads=[ps], writes=[pT])
                        for j in range(2):
                            for qs in range(NQS):
                                ab_, av = accv(j * NQS + qs)
                                last = (j == 1 and qs == NQS - 1)
                                op("pe", lambda e: e.matmul(av, lhsT=pT.t[:, j, qs * 128:(qs + 1) * 128], rhs=vb.t[:, kt, :],
                                                            start=(kt == 0 and (j * NQS + qs) % 3 == 0), stop=(kt == NKT - 1), skip_group_check=True),
                                   reads=[pT, vb], writes=[ab_], inc=last)
                    ost = ostr.next()
                    for qs in range(NQS):
                        b0, a0 = accv(qs)
                        b1, a1 = accv(NQS + qs)
                        rz = rzr.next(); tb = tbr.next(); db = dbr.next(); ss = ssr.next(); sj = sjr.next(); ob = obr.next()
                        op("dve", lambda e: e.reciprocal(out=rz.t[:, 0:1], in_=a0[:, 128:129]), reads=[b0], writes=[rz])
                        op("dve", lambda e: e.reciprocal(out=rz.t[:, 1:2], in_=a1[:, 128:129]), reads=[b1], writes=[rz])
                        op("dve", lambda e: e.tensor_tensor(out=rz.t[:, 1:2], in0=rz.t[:, 1:2], in1=lam.t[:, 2:3], op=ALU.mult),
                           reads=[rz, lam], writes=[rz])
                        op("dve", lambda e: e.tensor_scalar(out=tb.t[:], in0=a1[:, 0:128], scalar1=rz.t[:, 1:2], scalar2=None, op0=ALU.mult),
                           reads=[b1, rz], writes=[tb])
                        op("dve", lambda e: e.scalar_tensor_tensor(out=db.t[:], in0=a0[:, 0:128], scalar=rz.t[:, 0:1], in1=tb.t[:],
                                                                   op0=ALU.mult, op1=ALU.add), reads=[b0, rz, tb], writes=[db])
                        op("pool", lambda e: e.memset(ss.t[:], 0.0), writes=[ss])
                        op("act", lambda e: e.activation(out=sj.t[:], in_=db.t[:], func=AF.Square, accum_out=ss.t[:, 0:1]),
                           reads=[db, ss], writes=[sj, ss])
                        rsqrt_mean(ss.t[:, 0:1], ss.t[:, 0:1], 128, [ss], [ss])
                        op("dve", lambda e: e.scalar_tensor_tensor(out=ob.t[:], in0=db.t[:], scalar=ss.t[:, 0:1], in1=subg.t[:],
                                                                   op0=ALU.mult, op1=ALU.mult), reads=[db, ss, subg], writes=[ob])
                        op("pe", lambda e: e.transpose(out=ptb.t[:, 0, :], in_=ob.t[:], identity=ident), reads=[ob, cst], writes=[ptb])
                        op("act", lambda e: e.copy(out=ost.t[:, qs * 128:(qs + 1) * 128], in_=ptb.t[:, 0, :]), reads=[ptb], writes=[ost])
                    dma("act", lambda e: e.dma_start(out=obT_s[h * 128:(h + 1) * 128, qc * QC:(qc + 1) * QC], in_=ost.t[:]),
                        reads=[ost], writes=[obT_dep])
            sc.barrier()
        if stopat == 'E':
            sc.finalize("sp")
            return nc

        base = mk(es0, "base", [128, NE], F32)
        op("pool", lambda e: e.memset(base.t[:], 0.0), writes=[base])
        with contextlib.ExitStack() as es:
            def loadw(name, src_d, kch, ncol, q="pool"):
                wt = mk(es, name, [128, kch, ncol], BF16)
                for j in range(kch):
                    dma(q, lambda e: e.dma_start(out=wt.t[:, j, :], in_=src_d[j * 128:(j + 1) * 128, :]), writes=[wt])
                return wt
            wpa = loadw("wpa", wpa_d, 8, D)
            wpb = loadw("wpb", wpb_d, 8, D)
            wo = loadw("wo", wo_d, 8, D)
            wr = loadw("wr", wr_d, 8, NE)
            wds = loadw("wds", wds_d, 2, D)
            wgus = mk(es, "wgus", [128, 8, 512], BF16)
            for j in range(8):
                dma("pool", lambda e: e.dma_start(out=wgus.t[:, j, 0:256], in_=wgs_d[j * 128:(j + 1) * 128, :]), writes=[wgus])
                dma("pool", lambda e: e.dma_start(out=wgus.t[:, j, 256:512], in_=wus_d[j * 128:(j + 1) * 128, :]), writes=[wgus])
            g2bc = load_bc(es, "g2bc", 4)
            sh2bc = load_bc(es, "sh2bc", 5, "act")
            gate1 = load_bc(es, "gate1", 6)
            gate2 = load_bc(es, "gate2", 7, "act")
            rb = mk(es, "rb", [128, NE], F32)
            dma("sp", lambda e: e.dma_start(out=rb.t[:], in_=rb_d), writes=[rb])
            oaTr = mkring(es, "oaTf", [128, 8, 128], BF16, 2)
            obTr = mkring(es, "obTf", [128, 8, 128], BF16, 2)
            gtr = mkring(es, "gt", [128, 2 * D], BF16, 2)
            xr = mkring(es, "xf", [128, D], F32, 2)
            t1r = mkring(es, "t1f", [128, D], F32, 2)
            t2r = mkring(es, "t2f", [128, D], F32, 1)
            zr = mkring(es, "zf", [128, D], BF16, 2)
            zTr = mkring(es, "zTf", [128, 8, 128], BF16, 2)
            x1r = mkring(es, "x1f", [128, D], F32, 2)
            x1sr = mkring(es, "x1sf", [128, D], F32, 2)
            ssr = mkring(es, "ssf", [128, 1], F32, 2)
            junkr = mkring(es, "junkf", [128, D], BF16, 1)
            tmpr = mkring(es, "tmpf", [128, D], F32, 1)
            h2r = mkring(es, "h2f", [128, D], BF16, 2)
            h2Tr = mkring(es, "h2Tf", [128, 8, 128], BF16, 2)
            sgr = mkring(es, "sgf", [128, 256], F32, 2)
            abr = mkring(es, "abf", [128, 256], BF16, 2)
            aTr = mkring(es, "aTf", [128, 2, 128], BF16, 2)
            r256 = mkring(es, "r256", [128, NE], F32, 8)
            selbr = mkring(es, "selb", [128, NE], BF16, 2)
            r8 = mkring(es, "r8", [128, 8], F32, 8)
            big = mkpsring(es, "bigf", [128, D], F32, 2)
            ptr8 = mkpsring(es, "ptr8f", [128, 8, 128], BF16, 1)
            sm1 = mkps(es, "sm1", [128, 512], F32)
            sm2 = mkps(es, "sm2", [128, 512], F32)
            hid = mkps(es, "hidf", [128, 512], F32)
            x1_dep = Dep(); h2_dep = Dep(); rank_dep = Dep(); wt_dep = Dep()

            def mm8(pdst, lhsT, w, ncols, kch=8):
                for n in range(ncols // 512):
                    for j in range(kch):
                        op("pe", lambda e: e.matmul(pdst.t[:, n * 512:(n + 1) * 512], lhsT=lhsT.t[:, j, :], rhs=w.t[:, j, n * 512:(n + 1) * 512],
                                                    start=(j == 0), stop=(j == kch - 1)), reads=[lhsT, w], writes=[pdst],
                           inc=(j == kch - 1 and n == ncols // 512 - 1))

            for t in range(NT):
                rows = slice(t * 128, (t + 1) * 128)
                oaT = oaTr.next(); obT = obTr.next(); gt = gtr.next(); xt = xr.next()
                dma("sp", lambda e: e.dma_start(out=oaT.t[:], in_=oaT_s[:, rows].rearrange("(j p) n -> p j n", p=128)), writes=[oaT])
                dma("act", lambda e: e.dma_start(out=obT.t[:], in_=obT_s[:, rows].rearrange("(j p) n -> p j n", p=128)), writes=[obT])
                dma("sp", lambda e: e.dma_start(out=gt.t[:], in_=g_s[rows, :]), writes=[gt])
                dma("act", lambda e: e.dma_start(out=xt.t[:], in_=x_d[rows, :]), writes=[xt])
                pya = big.next(); mm8(pya, oaT, wpa, D)
                pyb = big.next(); mm8(pyb, obT, wpb, D)
                t1 = t1r.next(); t2 = t2r.next(); z = zr.next()
                op("dve", lambda e: e.tensor_tensor(out=t1.t[:], in0=pya.t[:], in1=gt.t[:, 0:D], op=ALU.mult), reads=[pya, gt], writes=[t1])
                op("dve", lambda e: e.tensor_tensor(out=t2.t[:], in0=pyb.t[:], in1=gt.t[:, D:2 * D], op=ALU.mult), reads=[pyb, gt], writes=[t2])
                op("pool", lambda e: e.tensor_tensor(out=z.t[:], in0=t1.t[:], in1=t2.t[:], op=ALU.add), reads=[t1, t2], writes=[z])
                zT = transpose8(z, ptr8, zTr)
                pmx = big.next(); mm8(pmx, zT, wo, D)
                t1 = t1r.next(); x1 = x1r.next()
                op("dve", lambda e: e.tensor_tensor(out=t1.t[:], in0=pmx.t[:], in1=gate1.t[:], op=ALU.mult), reads=[pmx, gate1], writes=[t1])
                op("pool", lambda e: e.tensor_tensor(out=x1.t[:], in0=t1.t[:], in1=xt.t[:], op=ALU.add), reads=[t1, xt], writes=[x1])
                h2 = norm_affine(x1, g2bc, sh2bc, ssr, junkr, tmpr, h2r)
                dma("sp", lambda e: e.dma_start(out=h2_s[rows, :], in_=h2.t[:]), reads=[h2], writes=[h2_dep])
                h2T = transpose8(h2, ptr8, h2Tr)
                for j in range(8):
                    op("pe", lambda e: e.matmul(hid.t[:], lhsT=h2T.t[:, j, :], rhs=wgus.t[:, j, :], start=(j == 0), stop=(j == 7)),
                       reads=[h2T, wgus], writes=[hid], inc=(j == 7))
                sg = sgr.next(); ab = abr.next(); aT = aTr.next()
                op("act", lambda e: e.activation(out=sg.t[:], in_=hid.t[:, 0:256], func=AF.Silu), reads=[hid], writes=[sg])
                op("dve", lambda e: e.tensor_tensor(out=ab.t[:], in0=sg.t[:], in1=hid.t[:, 256:512], op=ALU.mult), reads=[sg, hid], writes=[ab])
                pt = ptr8.next()
                for j in range(2):
                    op("pe", lambda e: e.transpose(out=pt.t[:, j, :], in_=ab.t[:, j * 128:(j + 1) * 128], identity=ident),
                       reads=[ab, cst], writes=[pt], inc=(j == 1))
                op("act", lambda e: e.copy(out=aT.t[:], in_=pt.t[:, 0:2, :]), reads=[pt], writes=[aT])
                pys = big.next(); mm8(pys, aT, wds, D, kch=2)
                t1 = t1r.next(); x1s = x1sr.next()
                op("dve", lambda e: e.tensor_tensor(out=t1.t[:], in0=pys.t[:], in1=gate2.t[:], op=ALU.mult), reads=[pys, gate2], writes=[t1])
                op("pool", lambda e: e.tensor_tensor(out=x1s.t[:], in0=t1.t[:], in1=x1.t[:], op=ALU.add), reads=[t1, x1], writes=[x1s])
                dma("act", lambda e: e.dma_start(out=x1_s[rows, :], in_=x1s.t[:]), reads=[x1s], writes=[x1_dep])
                for j in range(8):
                    op("pe", lambda e: e.matmul(sm1.t[:, 0:256], lhsT=h2T.t[:, j, :], rhs=wr.t[:, j, :], start=(j == 0), stop=(j == 7)),
                       reads=[h2T, wr], writes=[sm1], inc=(j == 7))
                scs = r256.next(); bia = r256.next(); mb = r256.next(); sel = r256.next(); wv = r256.next(); rk = r256.next()
                gs = r8.next(); m8 = r8.next(); g8 = r8.next(); gm = r8.next(); tn = r8.next(); t8 = r8.next(); ws = r8.next()
                op("act", lambda e: e.activation(out=scs.t[:], in_=sm1.t[:, 0:256], func=AF.Sigmoid), reads=[sm1], writes=[scs])
                op("dve", lambda e: e.tensor_tensor(out=bia.t[:], in0=scs.t[:], in1=rb.t[:], op=ALU.add), reads=[scs, rb], writes=[bia])
                for gi in range(8):
                    op("dve", lambda e: e.max(out=m8.t[:], in_=bia.t[:, gi * 32:(gi + 1) * 32]), reads=[bia], writes=[m8])
                    op("dve", lambda e: e.tensor_tensor(out=gs.t[:, gi:gi + 1], in0=m8.t[:, 0:1], in1=m8.t[:, 1:2], op=ALU.add),
                       reads=[m8], writes=[gs])
                op("dve", lambda e: e.max(out=g8.t[:], in_=gs.t[:]), reads=[gs], writes=[g8])
                op("dve", lambda e: e.tensor_scalar(out=gm.t[:], in0=gs.t[:], scalar1=g8.t[:, 3:4], scalar2=None, op0=ALU.is_ge),
                   reads=[gs, g8], writes=[gm])
                op("dve", lambda e: e.tensor_scalar(out=tn.t[:], in0=gm.t[:], scalar1=4.0, scalar2=-4.0, op0=ALU.mult, op1=ALU.add),
                   reads=[gm], writes=[tn])
                b3 = bia.t[:].rearrange("p (g k) -> p g k", k=32)
                m3 = mb.t[:].rearrange("p (g k) -> p g k", k=32)
                op("dve", lambda e: e.tensor_tensor(out=m3, in0=b3, in1=gm.t[:].unsqueeze(2).to_broadcast([128, 8, 32]), op=ALU.mult),
                   reads=[bia, gm], writes=[mb])
                op("dve", lambda e: e.tensor_tensor(out=m3, in0=m3, in1=tn.t[:].unsqueeze(2).to_broadcast([128, 8, 32]), op=ALU.add),
                   reads=[mb, tn], writes=[mb])
                op("dve", lambda e: e.max(out=t8.t[:], in_=mb.t[:]), reads=[mb], writes=[t8])
                op("dve", lambda e: e.tensor_scalar(out=sel.t[:], in0=mb.t[:], scalar1=t8.t[:, 7:8], scalar2=None, op0=ALU.is_ge),
                   reads=[mb, t8], writes=[sel])
                op("dve", lambda e: e.tensor_tensor(out=wv.t[:], in0=scs.t[:], in1=sel.t[:], op=ALU.mult), reads=[scs, sel], writes=[wv])
                op("dve", lambda e: e.tensor_reduce(out=ws.t[:, 0:1], in_=wv.t[:], axis=AX.X, op=ALU.add), reads=[wv], writes=[ws])
                op("dve", lambda e: e.reciprocal(out=ws.t[:, 0:1], in_=ws.t[:, 0:1]), reads=[ws], writes=[ws])
                op("dve", lambda e: e.tensor_scalar(out=wv.t[:], in0=wv.t[:], scalar1=ws.t[:, 0:1], scalar2=2.5, op0=ALU.mult, op1=ALU.mult),
                   reads=[wv, ws], writes=[wv])
                dma("sp", lambda e: e.dma_start(out=wt_s[rows, :], in_=wv.t[:]), reads=[wv], writes=[wt_dep])
                selb = selbr.next()
                op("pool", lambda e: e.tensor_copy(out=selb.t[:], in_=sel.t[:]), reads=[sel], writes=[selb])
                op("pe", lambda e: e.matmul(sm1.t[:, 256:512], lhsT=tri, rhs=selb.t[:], start=True, stop=True), reads=[selb, cst], writes=[sm1], inc=False)
                op("pe", lambda e: e.matmul(sm2.t[:, 0:256], lhsT=ones_bf, rhs=selb.t[:], start=True, stop=True), reads=[selb, cst], writes=[sm2])
                op("dve", lambda e: e.tensor_tensor(out=rk.t[:], in0=sm1.t[:, 256:512], in1=base.t[:], op=ALU.add), reads=[sm1, base], writes=[rk])
                dma("act", lambda e: e.dma_start(out=rank_s[rows, :], in_=rk.t[:]), reads=[rk], writes=[rank_dep])
                op("dve", lambda e: e.tensor_tensor(out=base.t[:], in0=sm2.t[:, 0:256], in1=base.t[:], op=ALU.add), reads=[sm2, base], writes=[base])
            sc.barrier()
        if stopat == 'F':
            sc.finalize("sp")
            return nc

        with contextlib.ExitStack() as es:
            gate2 = load_bc(es, "gate2g", 7, "act")
            thr = mk(es, "thr", [128, NB128], F32)
            dma("sp", lambda e: e.dma_start(out=thr.t[:], in_=thr_d), writes=[thr])
            qm = mk(es, "qm", [128, NE], F32)
            pad = mk(es, "pad", [128, NE], F32)
            cum = [mk(es, "cum%d" % i, [128, NE], F32) for i in range(2)]
            pstart = mk(es, "pstart", [128, NE], F32)
            op("dve", lambda e: e.memset(qm.t[:], 0.0), writes=[qm])
            for m_ in range(NT):
                op("dve", lambda e: e.scalar_tensor_tensor(out=qm.t[:], in0=base.t[:], scalar=128.0 * m_, in1=qm.t[:],
                                                           op0=ALU.is_gt, op1=ALU.add), reads=[base, qm], writes=[qm])
            op("dve", lambda e: e.tensor_scalar(out=pad.t[:], in0=qm.t[:], scalar1=128.0, scalar2=None, op0=ALU.mult), reads=[qm], writes=[pad])
            cur = pad
            s_ = 1
            k_ = 0
            while s_ < NE:
                nxt = cum[k_ % 2]
                op("dve", lambda e: e.tensor_copy(out=nxt.t[:, 0:s_], in_=cur.t[:, 0:s_]), reads=[cur], writes=[nxt])
                op("dve", lambda e: e.tensor_tensor(out=nxt.t[:, s_:NE], in0=cur.t[:, s_:NE], in1=cur.t[:, 0:NE - s_], op=ALU.add),
                   reads=[cur], writes=[nxt])
                cur = nxt
                s_ *= 2
                k_ += 1
            pend = cur
            op("dve", lambda e: e.tensor_tensor(out=pstart.t[:], in0=pend.t[:], in1=pad.t[:], op=ALU.subtract), reads=[pend, pad], writes=[pstart])
            eb = mk(es, "eb", [128, NB128], F32)
            ebrep = mk(es, "ebrep", [128, NB128 * 128], F32)
            pcol = mk(es, "pcol", [128, 1], F32)
            for c in range(NB128):
                op("dve", lambda e: e.tensor_scalar(out=qm.t[:], in0=pend.t[:], scalar1=thr.t[:, c:c + 1], scalar2=None, op0=ALU.is_le),
                   reads=[pend, thr], writes=[qm])
                op("dve", lambda e: e.tensor_reduce(out=eb.t[:, c:c + 1], in_=qm.t[:], axis=AX.X, op=ALU.add), reads=[qm], writes=[eb])
            eb_dep = Dep()
            dma("sp", lambda e: e.dma_start(out=eb_s[0].rearrange("(c p) -> p c", p=128), in_=eb.t[:], allow_slow_non_contiguous=True),
                reads=[eb], writes=[eb_dep])
            dma("sp", lambda e: e.dma_start(out=ebrep.t[:], in_=eb_s.partition_broadcast(128)), reads=[eb_dep], writes=[ebrep])
            op("dve", lambda e: e.tensor_scalar(out=pcol.t[:], in0=thr.t[:, 0:1], scalar1=1.0 / 128, scalar2=None, op0=ALU.mult),
               reads=[thr], writes=[pcol])
            idxf = mk(es, "idxf", [128, NBLK], F32)
            eqp = mk(es, "eqp", [128, NBLK], F32)
            op("dve", lambda e: e.tensor_scalar(out=idxf.t[:], in0=ebrep.t[:, 0:NBLK], scalar1=128.0, scalar2=pcol.t[:, 0:1],
                                                op0=ALU.mult, op1=ALU.add), reads=[ebrep, pcol], writes=[idxf])
            op("dve", lambda e: e.memset(eqp.t[:, 0:1], 0.0), writes=[eqp])
            op("dve", lambda e: e.tensor_tensor(out=eqp.t[:, 1:NBLK], in0=ebrep.t[:, 1:NBLK], in1=ebrep.t[:, 0:NBLK - 1], op=ALU.is_equal),
               reads=[ebrep], writes=[eqp])
            op("dve", lambda e: e.scalar_tensor_tensor(out=eqp.t[:], in0=idxf.t[:], scalar=-40000.0, in1=eqp.t[:], op0=ALU.add, op1=ALU.mult),
               reads=[idxf, eqp], writes=[eqp])
            op("dve", lambda e: e.tensor_tensor(out=idxw.t[:], in0=idxf.t[:], in1=eqp.t[:], op=ALU.subtract), reads=[idxf, eqp], writes=[idxw])
            op("pool", lambda e: e.memset(w8.t[:], 0.0), writes=[w8])
            rkr = mkring(es, "rkg", [128, NE], F32, 2)
            wvr = mkring(es, "wvg", [128, NE], F32, 2)
            h2r = mkring(es, "h2g", [128, D], BF16, 2)
            r256 = mkring(es, "r256g", [128, NE], F32, 4)
            d8r = mkring(es, "d8g", [128, 8], F32, 2)
            for t in range(NT):
                rows = slice(t * 128, (t + 1) * 128)
                rk = rkr.next(); wv = wvr.next(); h2 = h2r.next()
                dma("sp", lambda e: e.dma_start(out=rk.t[:], in_=rank_s[rows, :]), writes=[rk])
                dma("act", lambda e: e.dma_start(out=wv.t[:], in_=wt_s[rows, :]), writes=[wv])
                dma("sp", lambda e: e.dma_start(out=h2.t[:], in_=h2_s[rows, :]), writes=[h2])
                selm = r256.next(); dm = r256.next(); jk = r256.next(); d8 = d8r.next()
                op("dve", lambda e: e.tensor_scalar(out=selm.t[:], in0=wv.t[:], scalar1=0.0, scalar2=None, op0=ALU.is_gt), reads=[wv], writes=[selm])
                op("dve", lambda e: e.tensor_tensor(out=dm.t[:], in0=rk.t[:], in1=pstart.t[:], op=ALU.add), reads=[rk, pstart], writes=[dm])
                op("dve", lambda e: e.scalar_tensor_tensor(out=dm.t[:], in0=dm.t[:], scalar=1.0, in1=selm.t[:], op0=ALU.add, op1=ALU.mult),
                   reads=[dm, selm], writes=[dm])
                op("dve", lambda e: e.max(out=d8.t[:], in_=dm.t[:]), reads=[dm], writes=[d8])
                op("dve", lambda e: e.tensor_scalar(out=idx8.t[:, t, :], in0=d8.t[:], scalar1=-1.0, scalar2=None, op0=ALU.add),
                   reads=[d8], writes=[idx8])
                for k in range(8):
                    op("dve", lambda e: e.scalar_tensor_tensor(out=jk.t[:], in0=dm.t[:], scalar=d8.t[:, k:k + 1], in1=wv.t[:],
                                                               op0=ALU.is_equal, op1=ALU.mult, accum_out=w8.t[:, t, k:k + 1]),
                       reads=[dm, d8, wv, w8], writes=[jk, w8])
                for k in range(8):
                    dma("pool", lambda e: e.indirect_dma_start(
                        out=xg_s, out_offset=bass.IndirectOffsetOnAxis(ap=idx8.t[:, t, k:k + 1], axis=0), in_=h2.t[:], in_offset=None,
                        bounds_check=bnd_rows, oob_is_err=False), reads=[h2, idx8], writes=[xg_dep])
            xgr = mkring(es, "xgb", [128, D], BF16, 3)
            wgr = mkring(es, "wgb", [128, 8, 256], BF16, 1)
            wur = mkring(es, "wub", [128, 8, 256], BF16, 1)
            wdr = mkring(es, "wdb", [128, 2, D], BF16, 1)
            for rr in (wgr, wur, wdr):
                for b_ in rr.bufs:
                    op("pool", lambda e: e.memset(b_.t[:], 0.0), writes=[b_])
            xTr = mkring(es, "xTg", [128, 8, 128], BF16, 2)
            sgr = mkring(es, "sgg", [128, 256], F32, 2)
            abr = mkring(es, "abg", [128, 256], BF16, 2)
            aTr = mkring(es, "aTg", [128, 2, 128], BF16, 2)
            ystr = mkring(es, "ystg", [128, D], F32, 2)
            ptr8 = mkpsring(es, "ptr8g", [128, 8, 128], BF16, 1)
            ptr2 = mkpsring(es, "ptr2g", [128, 8, 128], BF16, 1)
            hidr = mkpsring(es, "hidg", [128, 512], F32, 2)
            pyr = mkpsring(es, "pyg", [128, D], F32, 2)
            ys_dep = Dep()
            import os as _os
            for b in range(min(NBLK, int(_os.environ.get('MAXBLK', '100000')))):
                xg = xgr.next(); wg = wgr.next(); wu = wur.next(); wd = wdr.next()
                dma("sp", lambda e: e.dma_start(out=xg.t[:], in_=xg_s[b * 128:(b + 1) * 128, :]), reads=[xg_dep], writes=[xg])
                for (wt_, src_) in ((wg, wge_d), (wu, wue_d), (wd, wde_d)):
                    dma("pool", lambda e: e.indirect_dma_start(
                        out=wt_.t[:].rearrange("p j n -> p (j n)"), out_offset=None, in_=src_,
                        in_offset=bass.IndirectOffsetOnAxis(ap=idxw.t[:, b:b + 1], axis=0),
                        bounds_check=bnd_w, oob_is_err=False), reads=[idxw], writes=[wt_])
                pt = ptr8.next(); xT = xTr.next()
                xv = xg.t[:].rearrange("p (q j) -> p j q", j=8)
                for j in range(8):
                    op("pe", lambda e: e.transpose(out=pt.t[:, j, :], in_=xv[:, j, :], identity=ident), reads=[xg, cst], writes=[pt], inc=(j == 7))
                op("act", lambda e: e.copy(out=xT.t[:], in_=pt.t[:]), reads=[pt], writes=[xT])
                hid = hidr.next()
                for j in range(8):
                    op("pe", lambda e: e.matmul(hid.t[:, 0:256], lhsT=xT.t[:, j, :], rhs=wg.t[:, j, :], start=(j == 0), stop=(j == 7), skip_group_check=True),
                       reads=[xT, wg], writes=[hid], inc=False)
                    op("pe", lambda e: e.matmul(hid.t[:, 256:512], lhsT=xT.t[:, j, :], rhs=wu.t[:, j, :], start=False, stop=(j == 7), skip_group_check=True),
                       reads=[xT, wu], writes=[hid], inc=(j == 7))
                sg = sgr.next(); ab = abr.next(); aT = aTr.next()
                op("act", lambda e: e.activation(out=sg.t[:], in_=hid.t[:, 0:256], func=AF.Silu), reads=[hid], writes=[sg])
                op("dve", lambda e: e.tensor_tensor(out=ab.t[:], in0=sg.t[:], in1=hid.t[:, 256:512], op=ALU.mult), reads=[sg, hid], writes=[ab])
                pt2 = ptr2.next()
                av = ab.t[:].rearrange("p (q j) -> p j q", j=2)
                for j in range(2):
                    op("pe", lambda e: e.transpose(out=pt2.t[:, j, :], in_=av[:, j, :], identity=ident), reads=[ab, cst], writes=[pt2], inc=(j == 1))
                op("dve", lambda e: e.tensor_copy(out=aT.t[:], in_=pt2.t[:, 0:2, :]), reads=[pt2], writes=[aT])
                py = pyr.next()
                for n in range(2):
                    for j in range(2):
                        op("pe", lambda e: e.matmul(py.t[:, n * 512:(n + 1) * 512], lhsT=aT.t[:, j, :], rhs=wd.t[:, j, n * 512:(n + 1) * 512],
                                                    start=(j == 0), stop=(j == 1)), reads=[aT, wd], writes=[py], inc=(j == 1 and n == 1))
                yst = ystr.next()
                if b % 2 == 0:
                    op("act", lambda e: e.copy(out=yst.t[:], in_=py.t[:]), reads=[py], writes=[yst])
                else:
                    op("dve", lambda e: e.tensor_copy(out=yst.t[:], in_=py.t[:]), reads=[py], writes=[yst])
                for hf in range(2):
                    dma("act" if hf == 0 else "sp", lambda e: e.dma_start(out=ys_s[hf][b * 128:(b + 1) * 128, :], in_=yst.t[:, hf * 512:(hf + 1) * 512]),
                        reads=[yst], writes=[ys_dep])
            gkr = mkring(es, "gkg", [128, D], F32, 3)
            for b_ in gkr.bufs:
                op("pool", lambda e: e.memset(b_.t[:], 0.0), writes=[b_])
            accr = mkring(es, "accg", [128, D], F32, 2)
            x1r = mkring(es, "x1g", [128, D], F32, 2)
            out_dep = Dep()
            for t in range(NT):
                rows = slice(t * 128, (t + 1) * 128)
                x1s = x1r.next(); acc = accr.next()
                dma("sp", lambda e: e.dma_start(out=x1s.t[:], in_=x1_s[rows, :]), writes=[x1s])
                for k in range(8):
                    gk = gkr.next()
                    for hf in range(2):
                        dma("pool", lambda e: e.indirect_dma_start(
                            out=gk.t[:, hf * 512:(hf + 1) * 512], out_offset=None, in_=ys_s[hf],
                            in_offset=bass.IndirectOffsetOnAxis(ap=idx8.t[:, t, k:k + 1], axis=0),
                            bounds_check=bnd_rows, oob_is_err=False), reads=[idx8, ys_dep], writes=[gk])
                    if k == 0:
                        op("dve", lambda e: e.tensor_scalar(out=acc.t[:], in0=gk.t[:], scalar1=w8.t[:, t, 0:1], scalar2=None, op0=ALU.mult),
                           reads=[gk, w8], writes=[acc])
                    else:
                        op("dve", lambda e: e.scalar_tensor_tensor(out=acc.t[:], in0=gk.t[:], scalar=w8.t[:, t, k:k + 1], in1=acc.t[:],
                                                                   op0=ALU.mult, op1=ALU.add), reads=[gk, w8, acc], writes=[acc])
                op("dve", lambda e: e.tensor_tensor(out=acc.t[:], in0=acc.t[:], in1=gate2.t[:], op=ALU.mult), reads=[acc, gate2], writes=[acc])
                op("pool", lambda e: e.tensor_tensor(out=acc.t[:], in0=acc.t[:], in1=x1s.t[:], op=ALU.add), reads=[acc, x1s], writes=[acc])
                dma("sp", lambda e: e.dma_start(out=out_d[rows, :], in_=acc.t[:]), reads=[acc], writes=[out_dep])
            sc.barrier()
        sc.finalize("sp")
    return nc


def _rope_tables(S):
    t = np.arange(S)
    row = (t // 64).astype(np.float32)
    col = (t % 64).astype(np.float32)
    freqs = (10000.0 ** (-np.arange(16, dtype=np.float32) / 16)).astype(np.float32)
    ar = row[:, None] * freqs[None, :]
    ac = col[:, None] * freqs[None, :]
    cr, sr, cc, sn = np.cos(ar), np.sin(ar), np.cos(ac), np.sin(ac)
    tab = np.zeros((S + CTX, 2, 64), np.float32)
    tab[:S, 0] = np.concatenate([cr, cr, cc, cc], 1)
    tab[:S, 1] = np.concatenate([-sr, sr, -sn, sn], 1)
    tab[S:, 0] = 1.0
    return tab


def _consts():
    k = np.arange(128)[:, None]
    q = np.arange(128)[None, :]
    c = np.zeros((128, 5, 128), np.float32)
    c[:, 0] = (k == q)
    c[:, 1] = (k >= q)
    c[:, 2] = (k <= q)
    c[:, 3] = (k < q)
    c[:, 4] = 1.0
    return c


def prep_core(inp, b, S):
    f = lambda a: np.ascontiguousarray(a, dtype=np.float32)
    rep = lambda v: f(np.broadcast_to(np.asarray(v).reshape(1, -1), (128, np.asarray(v).size)))
    NBLK = (S * 8) // 128 + NE
    NB128 = (NBLK + 127) // 128
    d = {}
    d["x"] = f(inp["x"][b])
    d["ctx"] = f(inp["ctx"][b])
    d["cvec"] = f(np.stack([inp["c"][b].reshape(8, 128).T, inp["c_ctx"].reshape(8, 128).T], -1))
    d["w_ada"] = f(inp["w_ada"][0])
    d["b_ada"] = f(inp["b_ada"][0].reshape(48, 128).T)
    d["n12"] = f(np.stack([inp["norm1_g"][0].reshape(8, 128).T, inp["norm2_g"][0].reshape(8, 128).T], 1))
    d["w_in"] = f(inp["w_in"][0])
    d["gain"] = rep(np.concatenate([np.tile(inp["knorm_a"][0], 2), np.tile(inp["knorm_b"][0], 16),
                                    np.tile(inp["qnorm_a"][0], 16), np.tile(inp["qnorm_b"][0], 16)]))
    d["rope"] = _rope_tables(S)
    d["sink"] = rep(inp["sink_a"][0].reshape(-1))
    d["lamv"] = f(rep(np.stack([inp["lam_q1"][0], inp["lam_k1"][0], inp["lam_q2"][0], inp["lam_k2"][0]]).reshape(-1)).reshape(128, 4, 64))
    d["subg"] = rep(inp["subln_g"][0])
    d["w_pa"] = f(inp["w_pa"][0])
    d["w_pb"] = f(inp["w_pb"][0])
    d["w_o"] = f(inp["w_o"][0])
    d["w_router"] = f(inp["w_router"][0])
    d["rbias"] = rep(inp["router_bias"][0])
    d["w_gate_e"] = f(inp["w_gate_e"][0]).reshape(NE * 128, 2048)
    d["w_up_e"] = f(inp["w_up_e"][0]).reshape(NE * 128, 2048)
    d["w_down_e"] = f(inp["w_down_e"][0]).reshape(NE * 128, 2048)
    d["w_gate_s"] = f(inp["w_gate_s"][0])
    d["w_up_s"] = f(inp["w_up_s"][0])
    d["w_down_s"] = f(inp["w_down_s"][0])
    d["consts"] = _consts()
    d["thr"] = f((128.0 * (np.arange(NB128)[None, :] * 128 + np.arange(128)[:, None])))
    return d


def kernel(**inputs):
    inp = {k: np.asarray(v) for k, v in inputs.items()}
    B, S, _ = inp["x"].shape
    nc = build(S)
    in_maps = [prep_core(inp, b, S) for b in range(B)]
    res = run_bass_kernel_spmd(nc, in_maps, core_ids=list(range(B)))
    return np.stack([np.asarray(r["out"], dtype=np.float32) for r in res.results], 0)
```
